# Optimizing a Trainium2 kernel written in Bass

```python
import jax, jax.numpy as jnp
from jax import lax
import numpy as np

D_MODEL = 1024
BATCH = 32
SEQ = 256
DEPTH = 2
DEC_BATCH = 2
DEC_SEQ = 1024
PAST_LEN = 256

GRID_W = 64
D_CONV = D_MODEL
D_RNN = D_MODEL
H_RNN = 16
HD_RNN = D_RNN // H_RNN
CONV_A_W = 3
CONV_B_W = 4
CONV_B_LEFT = 2
RGLRU_C = 8.0
D_FF = 2816
N_EXPERTS = 8
TOP_K = 2
N_DENSE = (DEPTH + 1) // 2
N_MOE = DEPTH // 2
N_MOD = 6
N_IN = 3 * D_CONV + 2 * D_RNN + 2 * D_MODEL
SPLITS = [D_CONV, 2 * D_CONV, 3 * D_CONV, 3 * D_CONV + D_RNN, 3 * D_CONV + 2 * D_RNN, 3 * D_CONV + 2 * D_RNN + D_MODEL]
DN_ALPHA = (2.0 * DEPTH) ** 0.25
DN_BETA = (8.0 * DEPTH) ** -0.25

kernel_name = "hybrid_shortconv_rglru_diffusion_step"


def _ln_plain(x, eps=1e-6):
    xf = x.astype(jnp.float32)
    mu = jnp.mean(xf, axis=-1, keepdims=True)
    var = jnp.mean(jnp.square(xf - mu), axis=-1, keepdims=True)
    return ((xf - mu) * lax.rsqrt(var + eps)).astype(x.dtype)


def _ln(x, g, b):
    return _ln_plain(x, 1e-5) * g + b


def _dwconv(x, w, left):
    k = w.shape[0]
    t = x.shape[-2]
    pad = [(0, 0)] * (x.ndim - 2) + [(left, k - 1 - left), (0, 0)]
    xp = jnp.pad(x, pad)
    out = xp[..., 0:t, :] * w[0]
    for j in range(1, k):
        out = out + xp[..., j:j + t, :] * w[j]
    return out


def _conv_seq(x, w, left, grid):
    if not grid:
        return _dwconv(x, w, left)
    b, t, ch = x.shape
    rows = t // GRID_W
    return _dwconv(x.reshape(b, rows, GRID_W, ch), w, left).reshape(b, t, ch)


def _linear_scan(a, bt, h0, reverse):
    def combine(p, q):
        return (p[0] * q[0], q[0] * p[1] + q[1])
    a_cum, b_cum = lax.associative_scan(combine, (a, bt), axis=1, reverse=reverse)
    return a_cum * h0[:, None, :] + b_cum


def _rglru_dir(xr, w_ga, b_ga, w_gx, b_gx, lam, h0, reverse):
    bsz, t, _ = xr.shape
    xh = xr.reshape(bsz, t, H_RNN, HD_RNN)
    r = jax.nn.sigmoid(jnp.einsum('bthi,hij->bthj', xh, w_ga).reshape(bsz, t, D_RNN) + b_ga)
    ig = jax.nn.sigmoid(jnp.einsum('bthi,hij->bthj', xh, w_gx).reshape(bsz, t, D_RNN) + b_gx)
    log_a = -RGLRU_C * r.astype(jnp.float32) * jax.nn.softplus(-lam.astype(jnp.float32))
    a = jnp.exp(log_a)
    bt = jnp.sqrt(-jnp.expm1(2.0 * log_a)) * (ig * xr).astype(jnp.float32)
    return _linear_scan(a, bt, h0.astype(jnp.float32), reverse)


def _token_mixer(u, h0, grid, w_in, conv_a, w_a_out, conv_b, conv_b_bias, w_ga, b_ga, w_gx, b_gx, lam, w_b_out, w_o):
    proj = u @ w_in
    xa, bg, cg, y_r, x_r, g_a, g_b = jnp.split(proj, SPLITS, axis=-1)
    branch_a = (bg * _conv_seq(cg * xa, conv_a, 1, grid)) @ w_a_out
    xr = _conv_seq(x_r, conv_b, CONV_B_LEFT, grid) + conv_b_bias
    h_f = _rglru_dir(xr, w_ga[0], b_ga[0], w_gx[0], b_gx[0], lam[0], h0[:, 0], False)
    h_b = _rglru_dir(xr, w_ga[1], b_ga[1], w_gx[1], b_gx[1], lam[1], h0[:, 1], True)
    branch_b = ((h_f + h_b).astype(u.dtype) * jax.nn.gelu(y_r)) @ w_b_out
    merged = jax.nn.sigmoid(g_a) * branch_a + jax.nn.sigmoid(g_b) * branch_b
    state = jnp.stack([h_f[:, -1], h_b[:, 0]], axis=1)
    return merged @ w_o, state


def _swiglu(u, w1, w3, w2):
    return (jax.nn.silu(u @ w1) * (u @ w3)) @ w2


def _moe(u, w_r, b_r, we1, we3, we2):
    logits = (u @ w_r).astype(jnp.float32) + b_r.astype(jnp.float32)
    top_v, top_i = lax.top_k(logits, TOP_K)
    top_w = jax.nn.softmax(top_v, axis=-1)
    comb = jnp.sum(jax.nn.one_hot(top_i, N_EXPERTS, dtype=jnp.float32) * top_w[..., None], axis=-2).astype(u.dtype)
    out = jnp.zeros_like(u)
    for e in range(N_EXPERTS):
        out = out + comb[..., e:e + 1] * _swiglu(u, we1[e], we3[e], we2[e])
    return out


def _trunk(x, cond, h0_all, grid, params):
    (w_mod, b_mod, w_in, conv_a, w_a_out, conv_b, conv_b_bias, w_gate_a, b_gate_a, w_gate_x, b_gate_x,
     lru_lambda, w_b_out, w_o, ln1_g, ln1_b, ln2_g, ln2_b, ffn_w1, ffn_w3, ffn_w2,
     router_w, router_b, moe_w1, moe_w3, moe_w2) = params
    states = []
    for l in range(DEPTH):
        mod = (jax.nn.silu(cond) @ w_mod[l] + b_mod[l])[:, None, :]
        sh1, sc1, g1, sh2, sc2, g2 = jnp.split(mod, N_MOD, axis=-1)
        u = _ln_plain(x) * (1.0 + sc1) + sh1
        mix, st = _token_mixer(u, h0_all[:, l], grid, w_in[l], conv_a[l], w_a_out[l], conv_b[l], conv_b_bias[l],
                               w_gate_a[l], b_gate_a[l], w_gate_x[l], b_gate_x[l], lru_lambda[l], w_b_out[l], w_o[l])
        x = _ln(DN_ALPHA * x + g1 * mix, ln1_g[l], ln1_b[l])
        u2 = _ln_plain(x) * (1.0 + sc2) + sh2
        if l % 2 == 0:
            f = _swiglu(u2, ffn_w1[l // 2], ffn_w3[l // 2], ffn_w2[l // 2])
        else:
            f = _moe(u2, router_w[l // 2], router_b[l // 2], moe_w1[l // 2], moe_w3[l // 2], moe_w2[l // 2])
        x = _ln(DN_ALPHA * x + g2 * f, ln2_g[l], ln2_b[l])
        states.append(st)
    return x, jnp.stack(states, axis=1)


def setup_inputs(seed: int = 0) -> dict:
    key = jax.random.key(seed)
    ks = jax.random.split(key, 40)
    f32 = jnp.float32

    def nrm(k, shape, scale):
        return jax.random.normal(k, shape, f32) * scale

    a0 = jax.random.uniform(ks[16], (DEPTH, 2, D_RNN), f32, 0.9, 0.999)
    return {
        "x_prompt": nrm(ks[0], (BATCH, SEQ, D_MODEL), 1.0),
        "x_sample": nrm(ks[1], (DEC_BATCH, DEC_SEQ, D_MODEL), 1.0),
        "state_rglru": nrm(ks[2], (DEC_BATCH, DEPTH, 2, D_RNN), 0.5),
        "c": nrm(ks[3], (DEC_BATCH, D_MODEL), 1.0),
        "c_ctx": nrm(ks[4], (D_MODEL,), 1.0),
        "w_mod": nrm(ks[5], (DEPTH, D_MODEL, N_MOD * D_MODEL), 0.5 * D_MODEL ** -0.5),
        "b_mod": nrm(ks[6], (DEPTH, N_MOD * D_MODEL), 0.02),
        "w_in": nrm(ks[7], (DEPTH, D_MODEL, N_IN), D_MODEL ** -0.5),
        "conv_a": nrm(ks[8], (DEPTH, CONV_A_W, D_CONV), CONV_A_W ** -0.5),
        "w_a_out": nrm(ks[9], (DEPTH, D_CONV, D_MODEL), D_CONV ** -0.5),
        "conv_b": nrm(ks[10], (DEPTH, CONV_B_W, D_RNN), CONV_B_W ** -0.5),
        "conv_b_bias": nrm(ks[11], (DEPTH, D_RNN), 0.02),
        "w_gate_a": nrm(ks[12], (DEPTH, 2, H_RNN, HD_RNN, HD_RNN), HD_RNN ** -0.5),
        "b_gate_a": nrm(ks[13], (DEPTH, 2, D_RNN), 0.1),
        "w_gate_x": nrm(ks[14], (DEPTH, 2, H_RNN, HD_RNN, HD_RNN), HD_RNN ** -0.5),
        "b_gate_x": nrm(ks[15], (DEPTH, 2, D_RNN), 0.1),
        "lru_lambda": jnp.log(a0) - jnp.log1p(-a0),
        "w_b_out": nrm(ks[17], (DEPTH, D_RNN, D_MODEL), D_RNN ** -0.5),
        "w_o": nrm(ks[18], (DEPTH, D_MODEL, D_MODEL), DN_BETA * D_MODEL ** -0.5),
        "ln1_g": 1.0 + nrm(ks[19], (DEPTH, D_MODEL), 0.02),
        "ln1_b": nrm(ks[20], (DEPTH, D_MODEL), 0.02),
        "ln2_g": 1.0 + nrm(ks[21], (DEPTH, D_MODEL), 0.02),
        "ln2_b": nrm(ks[22], (DEPTH, D_MODEL), 0.02),
        "ffn_w1": nrm(ks[23], (N_DENSE, D_MODEL, D_FF), D_MODEL ** -0.5),
        "ffn_w3": nrm(ks[24], (N_DENSE, D_MODEL, D_FF), D_MODEL ** -0.5),
        "ffn_w2": nrm(ks[25], (N_DENSE, D_FF, D_MODEL), DN_BETA * D_FF ** -0.5),
        "router_w": nrm(ks[26], (N_MOE, D_MODEL, N_EXPERTS), D_MODEL ** -0.5),
        "router_b": nrm(ks[27], (N_MOE, N_EXPERTS), 0.01),
        "moe_w1": nrm(ks[28], (N_MOE, N_EXPERTS, D_MODEL, D_FF), D_MODEL ** -0.5),
        "moe_w3": nrm(ks[29], (N_MOE, N_EXPERTS, D_MODEL, D_FF), D_MODEL ** -0.5),
        "moe_w2": nrm(ks[30], (N_MOE, N_EXPERTS, D_FF, D_MODEL), DN_BETA * D_FF ** -0.5),
    }


def reference(x_prompt, x_sample, state_rglru, c, c_ctx, w_mod, b_mod, w_in, conv_a, w_a_out, conv_b, conv_b_bias,
              w_gate_a, b_gate_a, w_gate_x, b_gate_x, lru_lambda, w_b_out, w_o, ln1_g, ln1_b, ln2_g, ln2_b,
              ffn_w1, ffn_w3, ffn_w2, router_w, router_b, moe_w1, moe_w3, moe_w2):
    params = (w_mod, b_mod, w_in, conv_a, w_a_out, conv_b, conv_b_bias, w_gate_a, b_gate_a, w_gate_x, b_gate_x,
              lru_lambda, w_b_out, w_o, ln1_g, ln1_b, ln2_g, ln2_b, ffn_w1, ffn_w3, ffn_w2,
              router_w, router_b, moe_w1, moe_w3, moe_w2)
    h0_ctx = jnp.zeros((x_prompt.shape[0], DEPTH, 2, D_RNN), jnp.float32)
    y_prompt, ctx_state = _trunk(x_prompt, c_ctx[None, :], h0_ctx, False, params)
    new_state_rglru = ctx_state.astype(x_prompt.dtype)
    y_sample, _ = _trunk(x_sample, c, state_rglru, True, params)
    return (y_prompt, y_sample, new_state_rglru)
```

```python
import numpy as np
import concourse.bass as bass
import concourse.mybir as mybir
from concourse.bass_utils import run_bass_kernel_spmd

F32 = mybir.dt.float32
BF16 = mybir.dt.bfloat16
AF = mybir.ActivationFunctionType
ALU = mybir.AluOpType
AX = mybir.AxisListType

D = 1024
NTOK = 1280
NTB = 10
NCH = 8
DFF = 2816
NFC = 22
NE = 8
L = 2
NIN = 7168
ALPHA = (2.0 * L) ** 0.25
ENG = ['pe', 'act', 'dve', 'pool', 'sp']
TILES = [(0, 512), (512, 512), (1024, 256)]


class Rec:
    def __init__(self):
        self.call = None

    def __getattr__(self, name):
        def f(*a, **kw):
            self.call = (name, a, kw)
            return self
        return f


class Prog:
    def __init__(self):
        self.q = {e: [] for e in ENG}
        self.cnt = {e: 0 for e in ENG}
        self.waited = {e: {} for e in ENG}
        self.lw = {}
        self.rd = {}
        self.dcnt = {}

    def _wait(self, e, tok):
        name, val = tok
        if name == e and e == 'pe':
            return
        if self.waited[e].get(name, 0) >= val:
            return
        self.waited[e][name] = val
        self.q[e].append(('w', name, val))

    def deps(self, e, reads, writes):
        for k in reads:
            t = self.lw.get(k)
            if t:
                self._wait(e, t)
        for k in writes:
            t = self.lw.get(k)
            if t:
                self._wait(e, t)
            for n, v in self.rd.get(k, {}).items():
                self._wait(e, (n, v))

    def commit(self, tok, reads, writes):
        for k in reads:
            d = self.rd.setdefault(k, {})
            if d.get(tok[0], 0) < tok[1]:
                d[tok[0]] = tok[1]
        for k in writes:
            self.lw[k] = tok
            self.rd[k] = {}

    def op(self, e, fn, reads=(), writes=()):
        psr = [k for k in reads if isinstance(k, tuple) and k[0] == 'ps']
        if psr:
            reads = [k for k in reads if k not in psr]
            writes = list(writes) + psr
        self.deps(e, reads, writes)
        self.cnt[e] += 1
        tok = (e, self.cnt[e])
        rec = Rec()
        fn(rec)
        self.q[e].append(('i', rec.call))
        self.commit(tok, reads, writes)
        return tok

    def dma(self, e, semname, fn, reads=(), writes=()):
        self.deps(e, reads, writes)
        self.dcnt[semname] = self.dcnt.get(semname, 0) + 16
        tok = (semname, self.dcnt[semname])
        rec = Rec()
        fn(rec)
        self.q[e].append(('d', rec.call, semname))
        self.commit(tok, reads, writes)
        return tok

    def wait_tok(self, e, tok):
        self._wait(e, tok)

    def sem_names(self):
        return list(ENG) + sorted(self.dcnt.keys())

    def replay(self, e, eng, sems):
        for it in self.q[e]:
            if it[0] == 'w':
                eng.wait_ge(sems[it[1]], it[2])
            elif it[0] == 'i':
                name, a, kw = it[1]
                getattr(eng, name)(*a, **kw).then_inc(sems[e], 1)
            else:
                name, a, kw = it[1]
                getattr(eng, name)(*a, **kw).then_inc(sems[it[2]], 16)


def mk(ap, off, dims):
    return bass.AP(ap.tensor, ap.offset + off, [list(ap.ap[0])] + [list(d) for d in dims])


SP_BMOD = 0
SP_CA = 96
SP_CB = 144
SP_CBB = 208
SP_BGA = 224
SP_BGX = 256
SP_LAM = 288
SP_RB = 320
NS = 328
FL_KF = 0
FL_KB = 5
FL_CF = 10
NFL = 32
SLOT_BYTES = 12288
NSLOT = 3
QUARTERS = [(0, 6), (6, 6), (12, 6), (18, 4)]


class _Stop(Exception):
    pass


def build_nc(nl=L, stop=None):
    import contextlib
    nc = bass.Bass("TRN2", target_bir_lowering=False)

    def din(name, shape, dt=F32):
        return nc.dram_tensor(name, list(shape), dt, kind="ExternalInput").ap()

    def dout(name, shape, dt=F32):
        return nc.dram_tensor(name, list(shape), dt, kind="ExternalOutput").ap()

    xin = din("xin", [NTOK, D])
    cond_d = din("cond", [128, 16])
    h0_d = din("h0", [128, 160])
    flg_d = din("flags", [128, NFL])
    smallp_d = din("smallp", [128, NS])
    ident_d = din("ident", [128, 128])
    lnp_d = din("lnp", [L, 4, D])
    gwbd_d = din("gwbd", [L, 4, 8, 128, 128])
    w_mod_d = din("w_mod", [L, D, 6 * D])
    w_in_d = din("w_in", [L, D, NIN])
    w_a_out_d = din("w_a_out", [L, D, D])
    w_b_out_d = din("w_b_out", [L, D, D])
    w_o_d = din("w_o", [L, D, D])
    ffn_w1_d = din("ffn_w1", [1, D, DFF])
    ffn_w3_d = din("ffn_w3", [1, D, DFF])
    ffn_w2_d = din("ffn_w2", [1, DFF, D])
    router_w_d = din("router_w", [1, D, NE])
    moe_w1_d = din("moe_w1", [1, NE, D, DFF])
    moe_w3_d = din("moe_w3", [1, NE, D, DFF])
    moe_w2_d = din("moe_w2", [1, NE, DFF, D])
    yout = dout("yout", [NTOK, D])
    stout = dout("stout", [128, 160])

    P = Prog()
    es = contextlib.ExitStack()
    with es:
        def sb(name, n, dt=F32):
            return es.enter_context(nc.sbuf_tensor('s_' + name, [128, n], dt))

        xres_t = sb("xres", NTB * D)
        ubuf_t = sb("ubuf", NCH * NTOK, BF16)
        reg = sb("reg", 79360 // 2, BF16)
        ring_t = sb("ring", NSLOT * SLOT_BYTES // 2, BF16)
        tw = sb("tw", 5 * D)
        gbc = tw[:, 0:D]
        lnbc = tw[:, D:3 * D]
        tmA = tw[:, 3 * D:4 * D]
        xnbf = tw[:, 4 * D:5 * D].bitcast(BF16)
        smallp = sb("smallp", NS)
        flg = sb("flg", NFL)
        h0t = sb("h0t", 160)
        stbuf = sb("stbuf", 160)
        condt = sb("condt", 16)
        scond = sb("scond", 16, BF16)
        identf = sb("identf", 128)
        identb = sb("identb", 128, BF16)
        onesf = sb("onesf", 128)
        diag = sb("diag", 256)
        modT = sb("modT", 96 * L)
        cneg = sb("cneg", 64)
        lamt = sb("lamt", 64)
        wfl = sb("wfl", 100)
        fx = sb("fx", 256)
        mv_t = sb("mv", 2 * 4 * 16)
        rs_t = sb("rs", 2 * 16)
        lnst = {"n": 0, "rs": None}
        epsc = sb("epsc", 4)
        wrb = sb("wrb", 64, BF16)
        lg = sb("lg", 80)
        lg2 = sb("lg2", 80)
        eq1 = sb("eq1", 80)
        eq2 = sb("eq2", 80)
        comb = sb("comb", 80)
        mx = sb("mx", 40)
        ps = es.enter_context(nc.psum_tensor("ps", [128, 8, 512], F32))
        psb = ps[:].bitcast(BF16)

        def cf32(byte_off, n):
            return reg[:, byte_off // 2: byte_off // 2 + 2 * n].bitcast(F32)

        def cbf(byte_off, n):
            return reg[:, byte_off // 2: byte_off // 2 + n]

        xres = xres_t[:].rearrange("p (t f) -> p t f", t=NTB)
        ubuf = ubuf_t[:].rearrange("p (c t) -> p c t", c=NCH)
        a_pre = cbf(0, NCH * NTOK).rearrange("p (c t) -> p c t", c=NCH)
        b_pre = cbf(20480, NCH * NTOK).rearrange("p (c t) -> p c t", c=NCH)
        T = [cf32(40960 + i * 5120, NTOK) for i in range(7)] + [tw[:, i * NTOK:(i + 1) * NTOK] for i in range(3)]
        xrbf = cbf(76800, NTOK)
        merged = cbf(40960, NCH * NTOK).rearrange("p (c t) -> p c t", c=NCH)
        f_acc = cf32(0, NTB * D).rearrange("p (t f) -> p t f", t=NTB)
        actq = [cbf(40960 + i * 15360, 6 * NTOK).rearrange("p (c t) -> p c t", c=6) for i in range(2)]
        tS = [cf32(71680 + i * 2048, 512) for i in range(2)]

        pst = {'ptr': 0, 'reserved': set()}

        def palloc(n=1):
            p = pst['ptr']
            if p % n:
                p += n - p % n
            if p + n > 8:
                p = 0
            while n == 1 and p in pst['reserved']:
                p = (p + 1) % 8
            pst['ptr'] = (p + n) % 8
            return p

        def pk(b, n=1):
            return [('ps', b + i) for i in range(n)]

        pieces = []
        rst = {'issued': 0, 'used': 0}

        def slot_ap(i):
            s = i % NSLOT
            return ring_t[:, s * (SLOT_BYTES // 2):(s + 1) * (SLOT_BYTES // 2)]

        def ring_issue(upto):
            while rst['issued'] < min(upto, len(pieces)):
                i = rst['issued']
                tag, loader = pieces[i]
                s = i % NSLOT
                loader(slot_ap(i), ('ring', s), 'ring%d' % s)
                rst['issued'] += 1

        def ring_next(tag, hold=0):
            i = rst['used']
            assert pieces[i][0] == tag, (pieces[i][0], tag)
            ring_issue(i + NSLOT - hold)
            rst['used'] += 1
            return slot_ap(i), ('ring', i % NSLOT)

        def wdma(dst, src, key, sem):
            P.dma('pool', sem, lambda e: e.dma_start(out=dst, in_=src), writes=[key])

        def kview(w2d):
            return w2d.rearrange("(k p) n -> p k n", p=128)

        def mk_mod_piece(l, n):
            def ld(slot, key, sem):
                dst = slot[:, 0:8 * 512].rearrange("p (k n) -> p k n", k=8)
                wdma(dst, kview(w_mod_d[l])[:, :, n * 512:(n + 1) * 512], key, sem)
            return ld

        def mk_p1_piece(l, c):
            def ld(slot, key, sem):
                wv = kview(w_in_d[l])
                dst = slot[:, 0:5 * 1024].rearrange("p (g k n) -> p g k n", g=5, k=8)
                for gi, grp in enumerate([0, 2, 1, 4, 3]):
                    wdma(dst[:, gi], wv[:, :, grp * D + c * 128: grp * D + (c + 1) * 128], key, sem)
                gdst = slot[:, 5120:5120 + 512].rearrange("p (g n) -> p g n", g=4)
                wdma(gdst, gwbd_d[l, :, c].rearrange("g p n -> p g n"), key, sem)
            return ld

        def mk_p2_piece(l, m):
            def ld(slot, key, sem):
                wv = kview(w_in_d[l])
                dst = slot[:, 0:4 * 1024].rearrange("p (g k n) -> p g k n", g=4, k=8)
                wdma(dst[:, 0], wv[:, :, 5 * D + m * 128: 5 * D + (m + 1) * 128], key, sem)
                wdma(dst[:, 1], wv[:, :, 6 * D + m * 128: 6 * D + (m + 1) * 128], key, sem)
                wdma(dst[:, 2], kview(w_a_out_d[l])[:, :, m * 128:(m + 1) * 128], key, sem)
                wdma(dst[:, 3], kview(w_b_out_d[l])[:, :, m * 128:(m + 1) * 128], key, sem)
            return ld

        def mk_wo_piece(l, h):
            def ld(slot, key, sem):
                dst = slot[:, 0:4 * 1024].rearrange("p (k n) -> p k n", k=4)
                wdma(dst, kview(w_o_d[l])[:, 4 * h:4 * h + 4, :], key, sem)
            return ld

        def mk_f1_piece(w1, w3, f0, nf):
            def ld(slot, key, sem):
                dst = slot[:, 0:2 * 8 * 256].rearrange("p (g k n) -> p g k n", g=2, k=8)
                wdma(dst[:, 0, :, 0:nf * 128], kview(w1)[:, :, f0 * 128:(f0 + nf) * 128], key, sem)
                wdma(dst[:, 1, :, 0:nf * 128], kview(w3)[:, :, f0 * 128:(f0 + nf) * 128], key, sem)
            return ld

        def mk_f2_piece(w2, f0, nf):
            def ld(slot, key, sem):
                dst = slot[:, 0:nf * 1024].rearrange("p (k n) -> p k n", k=nf)
                wdma(dst, kview(w2)[:, f0:f0 + nf, :], key, sem)
            return ld

        def experts(l):
            if l % 2 == 0:
                return [(ffn_w1_d[l // 2], ffn_w3_d[l // 2], ffn_w2_d[l // 2])]
            return [(moe_w1_d[l // 2, e], moe_w3_d[l // 2, e], moe_w2_d[l // 2, e]) for e in range(NE)]

        def mod_jobs_at(site):
            kind, l = site[0], site[1]
            if kind == 'p1':
                return [(l, 4 + site[2])]
            if kind == 'p2' and l + 1 < nl and site[2] < 4:
                return [(l + 1, site[2])]
            return []

        for n in range(4):
            pieces.append((('mod', 0, n), mk_mod_piece(0, n)))
        for l in range(nl):
            for c in range(NCH):
                pieces.append((('p1', l, c), mk_p1_piece(l, c)))
                for (ml, n) in mod_jobs_at(('p1', l, c)):
                    pieces.append((('mod', ml, n), mk_mod_piece(ml, n)))
            for m in range(NCH):
                pieces.append((('p2', l, m), mk_p2_piece(l, m)))
                for (ml, n) in mod_jobs_at(('p2', l, m)):
                    pieces.append((('mod', ml, n), mk_mod_piece(ml, n)))
            for h in range(2):
                pieces.append((('wo', l, h), mk_wo_piece(l, h)))
            for e, (w1, w3, w2) in enumerate(experts(l)):
                for q, (f0, nf) in enumerate(QUARTERS):
                    for i in range(0, nf, 2):
                        pieces.append((('f1', l, e, q, i), mk_f1_piece(w1, w3, f0 + i, 2)))
                        for (ml, n) in mod_jobs_at(('f1', l, e, q, i)):
                            pieces.append((('mod', ml, n), mk_mod_piece(ml, n)))
                    pieces.append((('f2', l, e, q), mk_f2_piece(w2, f0, nf)))

        def spc(col, n=1):
            return smallp[:, col:col + n]

        def V(e):
            return e

        def dve(fn, r=(), w=()):
            return P.op('dve', fn, r, w)

        def act(fn, r=(), w=()):
            return P.op('act', fn, r, w)

        def pool(fn, r=(), w=()):
            return P.op('pool', fn, r, w)

        def pe(fn, r=(), w=()):
            return P.op('pe', fn, r, w)

        def kt(name):
            return [(name, j) for j in range(3)]

        P.dma('sp', 'ldx', lambda e: e.dma_start(out=xres, in_=xin.rearrange("(t p) f -> p t f", p=128)),
              writes=[('x', tb) for tb in range(NTB)])
        for ii, (dst, src, key) in enumerate([(smallp, smallp_d, 'smallp'), (flg, flg_d, 'flg'), (h0t, h0_d, 'h0'),
                                              (condt, cond_d, 'cond'), (identf, ident_d, 'identf')]):
            P.dma('sp', 'lds%d' % ii, lambda e, dst=dst, src=src: e.dma_start(out=dst[:], in_=src), writes=[key])
        wdma(wrb[:].rearrange("p (k n) -> p k n", k=8), kview(router_w_d[0]), 'wrb', 'ldr')
        act(lambda e: e.activation(out=scond[:], in_=condt[:], func=AF.Silu), ['cond'], ['scond'])
        dve(lambda e: e.tensor_copy(out=identb[:], in_=identf[:]), ['identf'], ['identb'])
        dve(lambda e: e.memset(onesf[:], 1.0), [], ['onesf'])
        dve(lambda e: e.memset(stbuf[:], 0.0), [], ['stbuf'])
        dve(lambda e: e.memset(epsc[:, 0:1], 1e-6), [], ['epsc'])
        dve(lambda e: e.memset(epsc[:, 1:2], 1e-5), [], ['epsc'])
        dve(lambda e: e.memset(epsc[:, 2:3], 1.0), [], ['epsc'])
        act(lambda e: e.activation(out=lamt[:, 0:32], in_=spc(SP_LAM, 32), func=AF.Exp, scale=-1.0), ['smallp'], ['lamt'])
        act(lambda e: e.activation(out=lamt[:, 32:64], in_=lamt[:, 0:32], func=AF.Ln, bias=epsc[:, 2:3], scale=1.0), ['lamt', 'epsc'], ['lamt'])
        dve(lambda e: e.tensor_scalar(out=cneg[:, 0:32], in0=lamt[:, 32:64], scalar1=-8.0, scalar2=None, op0=ALU.mult), ['lamt'], ['cneg'])
        dve(lambda e: e.tensor_scalar(out=cneg[:, 32:64], in0=lamt[:, 32:64], scalar1=-16.0, scalar2=None, op0=ALU.mult), ['lamt'], ['cneg'])

        out_toks = []

        def ln_stats_tile(tbs, eps_col, src_keyf):
            n = len(tbs)
            par = lnst["n"] % 2
            lnst["n"] += 1
            mv = mv_t[:, par * 64:(par + 1) * 64]
            rs = rs_t[:, par * 16:(par + 1) * 16]
            rsk = ("rs", par)
            lnst["rs"] = (rs, rsk)
            for i, tb in enumerate(tbs):
                o = i * 16
                dve(lambda e, tb=tb, o=o: e.bn_stats(out=mv[:, o:o + 6], in_=xres[:, tb, 0:512]), [src_keyf(tb)], [('mv', par, i)])
                dve(lambda e, tb=tb, o=o: e.bn_stats(out=mv[:, o + 6:o + 12], in_=xres[:, tb, 512:1024]), [src_keyf(tb)], [('mv', par, i)])
                dve(lambda e, o=o: e.bn_aggr(out=mv[:, o + 12:o + 14], in_=mv[:, o:o + 12]), [('mv', par, i)], [('mv', par, i)])
            mvv = mv.rearrange("p (i s) -> p i s", s=16)
            mvk = [('mv', par, i) for i in range(n)]
            act(lambda e: e.activation(out=rs[:, 0:n], in_=mvv[:, 0:n, 13], func=AF.Sqrt, bias=epsc[:, eps_col:eps_col + 1], scale=1.0),
                mvk + ['epsc'], [rsk])
            dve(lambda e: e.reciprocal(out=rs[:, 4:4 + n], in_=rs[:, 0:n]), [rsk], [rsk])
            dve(lambda e: e.scalar_tensor_tensor(out=rs[:, 8:8 + n], in0=mvv[:, 0:n, 12], scalar=-1.0, in1=rs[:, 4:4 + n],
                                                 op0=ALU.mult, op1=ALU.mult), mvk + [rsk], [rsk])

        def build_u(l, j, shc, scc, src_keyf):
            t0, tn = TILES[j]
            tbs = list(range(t0 // 128, (t0 + tn) // 128))
            slot = 0 if j < 2 else 1
            ln_stats_tile(tbs, 0, src_keyf)
            rs, rsk = lnst["rs"]
            b0 = palloc(4)
            for i, tb in enumerate(tbs):
                xb = xnbf[:, (i % 2) * D:(i % 2 + 1) * D]
                act(lambda e, xb=xb, tb=tb, i=i: e.activation(out=xb, in_=xres[:, tb, :], func=AF.Identity,
                                                             scale=rs[:, 4 + i:5 + i], bias=rs[:, 8 + i:9 + i]),
                    [src_keyf(tb), rsk], [('xnbf', i % 2)])
                for c in range(NCH):
                    pe(lambda e, xb=xb, c=c, i=i: e.transpose(out=psb[:, b0 + i, c * 128:(c + 1) * 128], in_=xb[:, c * 128:(c + 1) * 128],
                                                           identity=identb[:]),
                       [('xnbf', i % 2), 'identb'], pk(b0 + i))
            nb = len(tbs)
            hb = nb // 2
            for c in range(NCH):
                s1 = modT[:, l * 96 + (scc + c) * 2 + slot:l * 96 + (scc + c) * 2 + slot + 1]
                s2 = modT[:, l * 96 + (shc + c) * 2 + slot:l * 96 + (shc + c) * 2 + slot + 1]
                src = psb[:, b0:b0 + hb, c * 128:(c + 1) * 128]
                dst = ubuf[:, c, t0:t0 + hb * 128].rearrange("p (a b) -> p a b", a=hb)
                dve(lambda e, src=src, dst=dst, s1=s1, s2=s2: e.tensor_scalar(out=dst, in0=src, scalar1=s1, scalar2=s2,
                                                                            op0=ALU.mult, op1=ALU.add),
                    pk(b0, hb) + [('modT', l)], [('u', c, j, 0)])
                src = psb[:, b0 + hb:b0 + nb, c * 128:(c + 1) * 128]
                dst = ubuf[:, c, t0 + hb * 128:t0 + tn].rearrange("p (a b) -> p a b", a=nb - hb)
                act(lambda e, src=src, dst=dst, s1=s1, s2=s2: e.activation(out=dst, in_=src, func=AF.Identity, scale=s1, bias=s2),
                    pk(b0 + hb, nb - hb) + [('modT', l)], [('u', c, j, 1)])

        def build_gbc(l, chunk0, slot):
            b = palloc(2)
            for c in range(NCH):
                dg = diag[:, (c % 2) * 128:(c % 2 + 1) * 128]
                col = modT[:, l * 96 + (chunk0 + c) * 2 + slot:l * 96 + (chunk0 + c) * 2 + slot + 1]
                dve(lambda e, dg=dg, col=col: e.tensor_scalar(out=dg, in0=identf[:], scalar1=col, scalar2=None, op0=ALU.mult),
                    ['identf', ('modT', l)], [('diag', c % 2)])
                pe(lambda e, dg=dg, c=c: e.matmul(ps[:, b + c // 4, (c % 4) * 128:(c % 4 + 1) * 128], lhsT=onesf[:], rhs=dg, start=True, stop=True),
                   ['onesf', ('diag', c % 2)], pk(b + c // 4))
            act(lambda e: e.activation(out=gbc.rearrange("p (a b) -> p a b", a=2), in_=ps[:, b:b + 2, :], func=AF.Copy),
                pk(b, 2), ['gbc'])

        def load_lnbc(l, i0):
            for i in range(2):
                src = bass.AP(lnp_d.tensor, lnp_d[l, i0 + i, :].offset, [[0, 128], [1, D]])
                P.dma('sp', 'ldln%d' % i, lambda e, src=src, i=i: e.dma_start(out=lnbc[:, i * D:(i + 1) * D], in_=src), writes=[('lnbc', i)])

        def resid_ln(l, tbs, pkeys_f, src_ap_f, eps_col, last):
            for tb in tbs:
                dve(lambda e, tb=tb: e.tensor_tensor(out=tmA, in0=src_ap_f(tb), in1=gbc, op=ALU.mult),
                    pkeys_f(tb) + ['gbc'], ['tmA'])
                dve(lambda e, tb=tb: e.scalar_tensor_tensor(out=xres[:, tb, :], in0=xres[:, tb, :], scalar=float(ALPHA), in1=tmA,
                                                           op0=ALU.mult, op1=ALU.add), ['tmA', ('x', tb)], [('x', tb)])
            ln_stats_tile(tbs, eps_col, lambda tb: ('x', tb))
            rs, rsk = lnst["rs"]
            for i, tb in enumerate(tbs):
                act(lambda e, tb=tb, i=i: e.activation(out=xres[:, tb, :], in_=xres[:, tb, :], func=AF.Identity,
                                                       scale=rs[:, 4 + i:5 + i], bias=rs[:, 8 + i:9 + i]), [('x', tb), rsk], [('x', tb)])
                dve(lambda e, tb=tb: e.tensor_tensor(out=xres[:, tb, :], in0=xres[:, tb, :], in1=lnbc[:, 0:D], op=ALU.mult),
                    [('x', tb), ('lnbc', 0)], [('x', tb)])
                pool(lambda e, tb=tb: e.tensor_tensor(out=xres[:, tb, :], in0=xres[:, tb, :], in1=lnbc[:, D:2 * D], op=ALU.add),
                     [('x', tb), ('lnbc', 1)], [('x', tb)])
                if last:
                    tk = P.dma('sp', 'sty', lambda e, tb=tb: e.dma_start(out=yout[tb * 128:(tb + 1) * 128, :], in_=xres[:, tb, :]),
                               reads=[('x', tb)])
                    out_toks.append(tk)

        TWK = ['gbc', ('lnbc', 0), ('lnbc', 1), 'tmA']
        T79K = kt('T7') + kt('T8') + kt('T9')

        def barrier(keys, engines=('act', 'dve', 'pool', 'sp')):
            for en in engines:
                P.deps(en, [], keys)

        def chk(tag):
            if stop == tag:
                raise _Stop()

        def mod_job(ml, n):
            bank = palloc(1)
            slot, skey = ring_next(('mod', ml, n))
            wv = slot[:, 0:8 * 512].rearrange("p (k n) -> p k n", k=8)
            for f4 in range(4):
                for k in range(8):
                    pe(lambda e, wv=wv, f4=f4, k=k: e.matmul(ps[:, bank, f4 * 2:f4 * 2 + 2], lhsT=wv[:, k, f4 * 128:(f4 + 1) * 128],
                                                          rhs=scond[:, k * 2:k * 2 + 2], start=(k == 0), stop=(k == 7)),
                       [skey, 'scond'], pk(bank))
            c_lo = n * 4
            for s_ in range(2):
                dve(lambda e, s_=s_: e.tensor_tensor(out=mk(modT[:, 0:1], ml * 96 + c_lo * 2 + s_, [[2, 4]]),
                                                    in0=mk(ps[:, bank, 0:1], s_, [[2, 4]]),
                                                    in1=spc(SP_BMOD + ml * 48 + c_lo, 4), op=ALU.add), pk(bank) + ['smallp'], [('modT', ml)])
            if n in (2, 3, 8, 9):
                dve(lambda e: e.tensor_scalar(out=modT[:, ml * 96 + c_lo * 2:ml * 96 + c_lo * 2 + 8], in0=modT[:, ml * 96 + c_lo * 2:ml * 96 + c_lo * 2 + 8],
                                              scalar1=1.0, scalar2=None, op0=ALU.add), [('modT', ml)], [('modT', ml)])

        for n in range(4):
            mod_job(0, n)

        try:
            for l in range(nl):
                chk('mod')
                for j in range(3):
                    build_u(l, j, 0, 8, lambda tb: ('x', tb))
                chk('u')

                barrier(TWK, ('act', 'dve', 'pool'))
                for c in range(NCH):
                    slot, skey = ring_next(('p1', l, c))
                    wv = slot[:, 0:5 * 1024].rearrange("p (g k n) -> p g k n", g=5, k=8)
                    gwv = slot[:, 5120:5120 + 512].rearrange("p (g n) -> p g n", g=4)

                    def proj(gi):
                        banks = []
                        for j, (t0, tn) in enumerate(TILES):
                            b = palloc(1)
                            for k in range(8):
                                pe(lambda e, b=b, gi=gi, k=k, t0=t0, tn=tn: e.matmul(ps[:, b, 0:tn], lhsT=wv[:, gi, k, :], rhs=ubuf[:, k, t0:t0 + tn],
                                                                                 start=(k == 0), stop=(k == 7)),
                                   [skey, ('u', k, j, 0), ('u', k, j, 1)], pk(b))
                            banks.append(b)
                        return banks

                    def rows(t, lo, hi, r0=0, r1=20):
                        return mk(t[:, 0:1], r0 * 64 + lo, [[64, r1 - r0], [1, hi - lo]])

                    wa = [spc(SP_CA + (l * 3 + jj) * 8 + c) for jj in range(3)]
                    wb = [spc(SP_CB + (l * 4 + jj) * 8 + c) for jj in range(4)]
                    bx = proj(0)
                    for j, (t0, tn) in enumerate(TILES):
                        act(lambda e, j=j, t0=t0, tn=tn: e.activation(out=T[0][:, t0:t0 + tn], in_=ps[:, bx[j], 0:tn], func=AF.Copy),
                            pk(bx[j]), [('T0', j)])
                    bc_ = proj(1)
                    for j, (t0, tn) in enumerate(TILES):
                        dve(lambda e, j=j, t0=t0, tn=tn: e.tensor_tensor(out=T[1][:, t0:t0 + tn], in0=ps[:, bc_[j], 0:tn], in1=T[0][:, t0:t0 + tn],
                                                                      op=ALU.mult), pk(bc_[j]) + [('T0', j)], [('T1', j)])
                    for i, wcol in enumerate([wa[0], wa[2]]):
                        dve(lambda e, i=i, wcol=wcol: e.tensor_scalar(out=wfl[:, i * 20:(i + 1) * 20], in0=flg[:, FL_CF:FL_CF + 20], scalar1=wcol,
                                                                      scalar2=None, op0=ALU.mult), ['flg', 'smallp'], ['wfla'])
                    p_, acc = T[1], T[2]
                    dve(lambda e: e.tensor_scalar(out=acc[:, :], in0=p_[:, :], scalar1=wa[1], scalar2=None, op0=ALU.mult), kt('T1') + ['smallp'], kt('T2'))
                    dve(lambda e: e.scalar_tensor_tensor(out=rows(acc, 1, 64), in0=rows(p_, 0, 63), scalar=wa[0], in1=rows(acc, 1, 64),
                                                         op0=ALU.mult, op1=ALU.add), kt('T1') + kt('T2') + ['smallp'], kt('T2'))
                    dve(lambda e: e.scalar_tensor_tensor(out=rows(acc, 0, 63), in0=rows(p_, 1, 64), scalar=wa[2], in1=rows(acc, 0, 63),
                                                         op0=ALU.mult, op1=ALU.add), kt('T1') + kt('T2') + ['smallp'], kt('T2'))
                    dve(lambda e: e.tensor_tensor(out=fx[:, 0:19], in0=mk(p_[:, 0:1], 63, [[64, 19]]), in1=wfl[:, 1:20], op=ALU.mult),
                         kt('T1') + ['wfla'], ['fxa'])
                    dve(lambda e: e.tensor_tensor(out=mk(acc[:, 0:1], 64, [[64, 19]]), in0=mk(acc[:, 0:1], 64, [[64, 19]]), in1=fx[:, 0:19], op=ALU.add),
                         kt('T2') + ['fxa'], kt('T2'))
                    dve(lambda e: e.tensor_tensor(out=fx[:, 32:51], in0=mk(p_[:, 0:1], 64, [[64, 19]]), in1=wfl[:, 21:40], op=ALU.mult),
                         kt('T1') + ['wfla'], ['fxa'])
                    dve(lambda e: e.tensor_tensor(out=mk(acc[:, 0:1], 63, [[64, 19]]), in0=mk(acc[:, 0:1], 63, [[64, 19]]), in1=fx[:, 32:51], op=ALU.add),
                         kt('T2') + ['fxa'], kt('T2'))
                    br_ = proj(3)
                    for j, (t0, tn) in enumerate(TILES):
                        act(lambda e, j=j, t0=t0, tn=tn: e.activation(out=T[3][:, t0:t0 + tn], in_=ps[:, br_[j], 0:tn], func=AF.Copy),
                            pk(br_[j]), [('T3', j)])
                    for i, wcol in enumerate([wb[0], wb[1], wb[3]]):
                        dve(lambda e, i=i, wcol=wcol: e.tensor_scalar(out=wfl[:, (i + 2) * 20:(i + 3) * 20], in0=flg[:, FL_CF:FL_CF + 20], scalar1=wcol,
                                                                     scalar2=None, op0=ALU.mult), ['flg', 'smallp'], ['wflb'])
                    xq, xr = T[3], T[4]
                    dve(lambda e: e.tensor_scalar(out=xr[:, :], in0=xq[:, :], scalar1=wb[2], scalar2=spc(SP_CBB + l * 8 + c), op0=ALU.mult, op1=ALU.add),
                        kt('T3') + ['smallp'], kt('T4'))
                    for (sh, wcol) in [(2, wb[0]), (1, wb[1])]:
                        dve(lambda e, sh=sh, wcol=wcol: e.scalar_tensor_tensor(out=rows(xr, sh, 64), in0=rows(xq, 0, 64 - sh), scalar=wcol,
                                                                             in1=rows(xr, sh, 64), op0=ALU.mult, op1=ALU.add),
                            kt('T3') + kt('T4') + ['smallp'], kt('T4'))
                    dve(lambda e: e.scalar_tensor_tensor(out=rows(xr, 0, 63), in0=rows(xq, 1, 64), scalar=wb[3], in1=rows(xr, 0, 63),
                                                         op0=ALU.mult, op1=ALU.add), kt('T3') + kt('T4') + ['smallp'], kt('T4'))
                    dve(lambda e: e.tensor_tensor(out=fx[:, 64:102].rearrange("p (a b) -> p a b", b=2), in0=mk(xq[:, 0:1], 62, [[64, 19], [1, 2]]),
                                                  in1=mk(wfl[:, 0:1], 41, [[1, 19], [0, 2]]), op=ALU.mult), kt('T3') + ['wflb'], ['fxb'])
                    dve(lambda e: e.tensor_tensor(out=mk(xr[:, 0:1], 64, [[64, 19], [1, 2]]), in0=mk(xr[:, 0:1], 64, [[64, 19], [1, 2]]),
                                                  in1=fx[:, 64:102].rearrange("p (a b) -> p a b", b=2), op=ALU.add), kt('T4') + ['fxb'], kt('T4'))
                    dve(lambda e: e.tensor_tensor(out=fx[:, 128:147], in0=mk(xq[:, 0:1], 63, [[64, 19]]), in1=wfl[:, 61:80], op=ALU.mult),
                        kt('T3') + ['wflb'], ['fxb'])
                    dve(lambda e: e.tensor_tensor(out=mk(xr[:, 0:1], 64, [[64, 19]]), in0=mk(xr[:, 0:1], 64, [[64, 19]]), in1=fx[:, 128:147], op=ALU.add),
                        kt('T4') + ['fxb'], kt('T4'))
                    dve(lambda e: e.tensor_tensor(out=fx[:, 160:179], in0=mk(xq[:, 0:1], 64, [[64, 19]]), in1=wfl[:, 81:100], op=ALU.mult),
                        kt('T3') + ['wflb'], ['fxb'])
                    dve(lambda e: e.tensor_tensor(out=mk(xr[:, 0:1], 63, [[64, 19]]), in0=mk(xr[:, 0:1], 63, [[64, 19]]), in1=fx[:, 160:179], op=ALU.add),
                        kt('T4') + ['fxb'], kt('T4'))
                    act(lambda e: e.activation(out=xrbf[:, :], in_=xr[:, :], func=AF.Copy), kt('T4'), kt('xrbf'))
                    bb = proj(2)
                    for j, (t0, tn) in enumerate(TILES):
                        dve(lambda e, j=j, t0=t0, tn=tn: e.tensor_tensor(out=a_pre[:, c, t0:t0 + tn], in0=ps[:, bb[j], 0:tn], in1=acc[:, t0:t0 + tn],
                                                                      op=ALU.mult), pk(bb[j]) + [('T2', j)], [('apre', c, j)])
                    by = proj(4)
                    pst['reserved'] = set(by)
                    RT, IT, AT = (T[0], T[7]), (T[1], T[8]), (T[2], T[9])
                    RK, IK, AK = ('T0', 'T7'), ('T1', 'T8'), ('T2', 'T9')
                    HT, HK = (T[5], T[6]), ('T5', 'T6')
                    for d in range(2):
                        for g, dstT, dk, bcol in [(0, RT[d], RK[d], SP_BGA), (1, IT[d], IK[d], SP_BGX)]:
                            for j, (t0, tn) in enumerate(TILES):
                                b = palloc(1)
                                pe(lambda e, b=b, g=g, d=d, t0=t0, tn=tn: e.matmul(ps[:, b, 0:tn], lhsT=gwv[:, d * 2 + g, :], rhs=xrbf[:, t0:t0 + tn],
                                                                               start=True, stop=True), [skey, ('xrbf', j)], pk(b))
                                act(lambda e, b=b, dstT=dstT, t0=t0, tn=tn, bcol=bcol, d=d: e.activation(
                                    out=dstT[:, t0:t0 + tn], in_=ps[:, b, 0:tn], func=AF.Sigmoid,
                                    bias=spc(bcol + (l * 2 + d) * 8 + c), scale=1.0), pk(b) + ['smallp'], [(dk, j)])
                    for d in range(2):
                        cn = cneg[:, (l * 2 + d) * 8 + c:(l * 2 + d) * 8 + c + 1]
                        act(lambda e, d=d, cn=cn: e.activation(out=AT[d][:, :], in_=RT[d][:, :], func=AF.Exp, scale=cn), kt(RK[d]) + ['cneg'], kt(AK[d]))
                    for d in range(2):
                        dve(lambda e, d=d: e.tensor_tensor(out=RT[d][:, :], in0=AT[d][:, :], in1=AT[d][:, :], op=ALU.mult), kt(AK[d]), kt(RK[d]))
                    for d in range(2):
                        dve(lambda e, d=d: e.tensor_tensor(out=IT[d][:, :], in0=IT[d][:, :], in1=xr[:, :], op=ALU.mult), kt(IK[d]) + kt('T4'), kt(IK[d]))
                    for d in range(2):
                        act(lambda e, d=d: e.activation(out=RT[d][:, :], in_=RT[d][:, :], func=AF.Sqrt, scale=-1.0, bias=epsc[:, 2:3]),
                            kt(RK[d]) + ['epsc'], kt(RK[d]))
                    for j, (t0, tn) in enumerate(TILES):
                        act(lambda e, j=j, t0=t0, tn=tn: e.activation(out=T[3][:, t0:t0 + tn], in_=ps[:, by[j], 0:tn], func=AF.Gelu_apprx_tanh),
                            pk(by[j]), [('T3', j)])
                    pst['reserved'] = set()
                    for d in range(2):
                        aT, iT, hT = AT[d], IT[d], HT[d]
                        dve(lambda e, d=d: e.tensor_tensor(out=IT[d][:, :], in0=IT[d][:, :], in1=RT[d][:, :], op=ALU.mult), kt(IK[d]) + kt(RK[d]), kt(IK[d]))
                        col0 = 0 if d == 0 else 255
                        acols = mk(aT[:, 0:1], col0, [[256, 5]])
                        bcols = mk(iT[:, 0:1], col0, [[256, 5]])
                        h0v = h0t[:, ((l * 2 + d) * 8 + c) * 5:((l * 2 + d) * 8 + c) * 5 + 5]
                        kf = flg[:, (FL_KF if d == 0 else FL_KB):(FL_KF if d == 0 else FL_KB) + 5]
                        fo = 200 + d * 8
                        dve(lambda e, fo=fo: e.tensor_tensor(out=fx[:, fo:fo + 5], in0=acols, in1=h0v, op=ALU.mult), kt(AK[d]) + ['h0'], [('fxs', d)])
                        dve(lambda e, fo=fo: e.tensor_tensor(out=bcols, in0=bcols, in1=fx[:, fo:fo + 5], op=ALU.add), kt(IK[d]) + [('fxs', d)], kt(IK[d]))
                        dve(lambda e: e.tensor_tensor(out=acols, in0=acols, in1=kf, op=ALU.mult), kt(AK[d]) + ['flg'], kt(AK[d]))
                        if d == 0:
                            dve(lambda e: e.tensor_tensor_scan(out=hT[:, :], data0=aT[:, :], data1=iT[:, :], initial=0.0, op0=ALU.mult, op1=ALU.add),
                                kt(IK[d]) + kt(AK[d]), kt(HK[d]))
                        else:
                            rv = lambda t: mk(t[:, 0:1], NTOK - 1, [[-1, NTOK]])
                            dve(lambda e: e.tensor_tensor_scan(out=rv(hT), data0=rv(aT), data1=rv(iT), initial=0.0, op0=ALU.mult, op1=ALU.add),
                                kt(IK[d]) + kt(AK[d]), kt(HK[d]))
                        so = ((l * 2 + d) * 8 + c) * 5
                        act(lambda e, so=so, d=d: e.activation(out=stbuf[:, so:so + 5], in_=mk(HT[d][:, 0:1], 255 if d == 0 else 0, [[256, 5]]), func=AF.Copy),
                            kt(HK[d]), ['stbuf'])
                    dve(lambda e: e.tensor_tensor(out=T[5][:, :], in0=T[5][:, :], in1=T[6][:, :], op=ALU.add), kt('T5') + kt('T6'), kt('T5'))
                    dve(lambda e: e.tensor_tensor(out=b_pre[:, c, :], in0=T[5][:, :], in1=T[3][:, :], op=ALU.mult),
                         kt('T5') + kt('T3'), [('bpre', c, j) for j in range(3)])
                    for (ml, n) in mod_jobs_at(('p1', l, c)):
                        mod_job(ml, n)

                barrier(T79K)
                chk('p1')
                for m in range(NCH):
                    slot, skey = ring_next(('p2', l, m))
                    wv = slot[:, 0:4 * 1024].rearrange("p (g k n) -> p g k n", g=4, k=8)
                    for j, (t0, tn) in enumerate(TILES):
                        bs = []
                        for gi in range(4):
                            b = palloc(1)
                            src, skn = (ubuf, 'u') if gi < 2 else ((a_pre, 'apre') if gi == 2 else (b_pre, 'bpre'))
                            for k in range(8):
                                pe(lambda e, b=b, gi=gi, k=k, src=src, t0=t0, tn=tn: e.matmul(ps[:, b, 0:tn], lhsT=wv[:, gi, k, :], rhs=src[:, k, t0:t0 + tn],
                                                                                          start=(k == 0), stop=(k == 7)), [skey] + ([('u', k, j, 0), ('u', k, j, 1)] if skn == 'u' else [(skn, k, j)]), pk(b))
                            bs.append(b)
                        sa, sb_ = T[4][:, t0:t0 + tn], T[5][:, t0:t0 + tn]
                        act(lambda e, b=bs[0], sa=sa, tn=tn: e.activation(out=sa, in_=ps[:, b, 0:tn], func=AF.Sigmoid), pk(bs[0]), [('T4', j)])
                        act(lambda e, b=bs[1], sb_=sb_, tn=tn: e.activation(out=sb_, in_=ps[:, b, 0:tn], func=AF.Sigmoid), pk(bs[1]), [('T5', j)])
                        dve(lambda e, b=bs[2], sa=sa, tn=tn: e.tensor_tensor(out=sa, in0=ps[:, b, 0:tn], in1=sa, op=ALU.mult), pk(bs[2]) + [('T4', j)], [('T4', j)])
                        dve(lambda e, b=bs[3], sb_=sb_, tn=tn: e.tensor_tensor(out=sb_, in0=ps[:, b, 0:tn], in1=sb_, op=ALU.mult), pk(bs[3]) + [('T5', j)], [('T5', j)])
                        dve(lambda e, sa=sa, sb_=sb_, t0=t0, tn=tn: e.tensor_tensor(out=merged[:, m, t0:t0 + tn], in0=sa, in1=sb_, op=ALU.add),
                            [('T4', j), ('T5', j)], [('mg', m, j)])
                    for (ml, n) in mod_jobs_at(('p2', l, m)):
                        mod_job(ml, n)

                chk('p2')
                load_lnbc(l, 0)
                wo = []
                for h in range(2):
                    slot, skey = ring_next(('wo', l, h), hold=h)
                    wo.append((slot[:, 0:4 * 1024].rearrange("p (k n) -> p k n", k=4), skey))
                for j, (t0, tn) in enumerate(TILES):
                    tbs = list(range(t0 // 128, (t0 + tn) // 128))
                    if j == 0 or j == 2:
                        build_gbc(l, 16, 0 if j == 0 else 1)
                    pb = {}
                    for tb in tbs:
                        b = palloc(2)
                        pb[tb] = b
                        for k in range(8):
                            wv, skey = wo[k // 4]
                            for hf in range(2):
                                pe(lambda e, b=b, k=k, hf=hf, tb=tb, wv=wv: e.matmul(ps[:, b + hf, :], lhsT=merged[:, k, tb * 128:(tb + 1) * 128],
                                                                                  rhs=wv[:, k % 4, hf * 512:(hf + 1) * 512], start=(k == 0), stop=(k == 7)),
                                   [skey, ('mg', k, j)], pk(b + hf))
                    resid_ln(l, tbs, lambda tb: pk(pb[tb], 2), lambda tb: ps[:, pb[tb]:pb[tb] + 2, :].rearrange("p a b -> p (a b)"), 1, False)
                    build_u(l, j, 24, 32, lambda tb: ('x', tb))

                chk('p3')
                load_lnbc(l, 2)
                exps = experts(l)
                moe = len(exps) > 1
                if moe:
                    bl = palloc(1)
                    wrv = wrb[:].rearrange("p (k n) -> p k n", k=8)
                    for tb in range(NTB):
                        for k in range(8):
                            pe(lambda e, tb=tb, k=k: e.matmul(ps[:, bl, tb * 8:(tb + 1) * 8], lhsT=ubuf[:, k, tb * 128:(tb + 1) * 128], rhs=wrv[:, k, :],
                                                              start=(k == 0), stop=(k == 7)), ['wrb', ('u', k, tb // 4, 0), ('u', k, tb // 4, 1)], pk(bl))
                    v3 = lambda t: t[:].rearrange("p (a b) -> p a b", b=8)
                    bc3 = lambda t, o=0: mk(t[:, 0:1], o, [[1, 10], [0, 8]])
                    dve(lambda e: e.tensor_tensor(out=v3(lg), in0=ps[:, bl, 0:80].rearrange("p (a b) -> p a b", b=8),
                                                  in1=mk(smallp[:, 0:1], SP_RB, [[0, 10], [1, 8]]), op=ALU.add), pk(bl) + ['smallp'], ['lg'])
                    dve(lambda e: e.tensor_reduce(out=mx[:, 0:10], in_=v3(lg), axis=AX.X, op=ALU.max), ['lg'], ['mx'])
                    dve(lambda e: e.tensor_tensor(out=v3(eq1), in0=v3(lg), in1=bc3(mx, 0), op=ALU.is_equal), ['lg', 'mx'], ['eq1'])
                    dve(lambda e: e.scalar_tensor_tensor(out=lg2[:], in0=eq1[:], scalar=-1e30, in1=lg[:], op0=ALU.mult, op1=ALU.add), ['eq1', 'lg'], ['lg2'])
                    dve(lambda e: e.tensor_reduce(out=mx[:, 10:20], in_=v3(lg2), axis=AX.X, op=ALU.max), ['lg2'], ['mx'])
                    dve(lambda e: e.tensor_tensor(out=v3(eq2), in0=v3(lg2), in1=bc3(mx, 10), op=ALU.is_equal), ['lg2', 'mx'], ['eq2'])
                    dve(lambda e: e.tensor_tensor(out=mx[:, 20:30], in0=mx[:, 0:10], in1=mx[:, 10:20], op=ALU.subtract), ['mx'], ['mx'])
                    act(lambda e: e.activation(out=mx[:, 30:40], in_=mx[:, 20:30], func=AF.Sigmoid, scale=-1.0), ['mx'], ['mx'])
                    act(lambda e: e.activation(out=mx[:, 20:30], in_=mx[:, 20:30], func=AF.Sigmoid), ['mx'], ['mx'])
                    dve(lambda e: e.tensor_tensor(out=v3(eq1), in0=v3(eq1), in1=bc3(mx, 20), op=ALU.mult), ['eq1', 'mx'], ['eq1'])
                    dve(lambda e: e.tensor_tensor(out=v3(eq2), in0=v3(eq2), in1=bc3(mx, 30), op=ALU.mult), ['eq2', 'mx'], ['eq2'])
                    dve(lambda e: e.tensor_tensor(out=comb[:], in0=eq1[:], in1=eq2[:], op=ALU.add), ['eq1', 'eq2'], ['comb'])
                first = True
                qi = 0

                def final_tile(j):
                    t0, tn = TILES[j]
                    tbs = list(range(t0 // 128, (t0 + tn) // 128))
                    if j == 0 or j == 2:
                        build_gbc(l, 40, 0 if j == 0 else 1)
                    resid_ln(l, tbs, lambda tb: [('facc', tb)], lambda tb: f_acc[:, tb, :], 1, l == nl - 1)

                for ei, (w1, w3, w2) in enumerate(exps):
                    for q, (f0, nf) in enumerate(QUARTERS):
                        lastq = (ei == len(exps) - 1) and (q == len(QUARTERS) - 1)
                        aq = actq[qi % 2]
                        aqk = 'aq%d' % (qi % 2)
                        qi += 1
                        for i in range(0, nf, 2):
                            slot, skey = ring_next(('f1', l, ei, q, i))
                            wv = slot[:, 0:2 * 8 * 256].rearrange("p (g k n) -> p g k n", g=2, k=8)
                            for fi in range(2):
                                for j, (t0, tn) in enumerate(TILES):
                                    b1 = palloc(1)
                                    b3 = palloc(1)
                                    for g, b in ((0, b1), (1, b3)):
                                        for k in range(8):
                                            pe(lambda e, b=b, g=g, k=k, fi=fi, t0=t0, tn=tn, wv=wv: e.matmul(
                                                ps[:, b, 0:tn], lhsT=wv[:, g, k, fi * 128:(fi + 1) * 128], rhs=ubuf[:, k, t0:t0 + tn],
                                                start=(k == 0), stop=(k == 7)), [skey, ('u', k, j, 0), ('u', k, j, 1)], pk(b))
                                    ts_ = tS[(fi * 3 + j) % 2]
                                    tsk = ('tS', (fi * 3 + j) % 2)
                                    act(lambda e, b1=b1, ts_=ts_, tn=tn: e.activation(out=ts_[:, 0:tn], in_=ps[:, b1, 0:tn], func=AF.Silu), pk(b1), [tsk])
                                    dve(lambda e, b3=b3, ts_=ts_, t0=t0, tn=tn, aq=aq, fl=i + fi: e.tensor_tensor(
                                        out=aq[:, fl, t0:t0 + tn], in0=ps[:, b3, 0:tn], in1=ts_[:, 0:tn], op=ALU.mult), pk(b3) + [tsk], [(aqk, i + fi, j)])
                            for (ml, n) in mod_jobs_at(('f1', l, ei, q, i)):
                                mod_job(ml, n)
                        slot, skey = ring_next(('f2', l, ei, q))
                        w2v = slot[:, 0:nf * 1024].rearrange("p (k n) -> p k n", k=nf)
                        for tb in range(NTB):
                            b = palloc(2)
                            for k in range(nf):
                                for hf in range(2):
                                    pe(lambda e, b=b, k=k, hf=hf, tb=tb, aq=aq, w2v=w2v: e.matmul(
                                        ps[:, b + hf, :], lhsT=aq[:, k, tb * 128:(tb + 1) * 128], rhs=w2v[:, k, hf * 512:(hf + 1) * 512],
                                        start=(k == 0), stop=(k == nf - 1)), [skey, (aqk, k, tb // 4)], pk(b + hf))
                            src = ps[:, b:b + 2, :].rearrange("p a b -> p (a b)")
                            if first:
                                if moe:
                                    dve(lambda e, src=src, tb=tb, ei=ei: e.tensor_scalar(out=f_acc[:, tb, :], in0=src, scalar1=comb[:, tb * 8 + ei:tb * 8 + ei + 1],
                                                                                         scalar2=None, op0=ALU.mult), pk(b, 2) + ['comb'], [('facc', tb)])
                                else:
                                    dve(lambda e, src=src, tb=tb: e.tensor_copy(out=f_acc[:, tb, :], in_=src), pk(b, 2), [('facc', tb)])
                            else:
                                sc_ = comb[:, tb * 8 + ei:tb * 8 + ei + 1] if moe else 1.0
                                dve(lambda e, src=src, tb=tb, sc_=sc_: e.scalar_tensor_tensor(out=f_acc[:, tb, :], in0=src, scalar=sc_, in1=f_acc[:, tb, :],
                                                                                             op0=ALU.mult, op1=ALU.add),
                                    pk(b, 2) + ['comb', ('facc', tb)], [('facc', tb)])
                            if lastq and tb in (3, 7, 9):
                                final_tile({3: 0, 7: 1, 9: 2}[tb])
                        first = False

        except _Stop:
            for tb in range(NTB):
                tk = P.dma('sp', 'sty', lambda e, tb=tb: e.dma_start(out=yout[tb * 128:(tb + 1) * 128, :], in_=xres[:, tb, :]), reads=[('x', tb)])
                out_toks.append(tk)

        tk = P.dma('sp', 'stst', lambda e: e.dma_start(out=stout, in_=stbuf[:]), reads=['stbuf'])
        out_toks.append(tk)
        final = {}
        for (n, v) in out_toks:
            final[n] = max(final.get(n, 0), v)
        for n, v in final.items():
            P.wait_tok('sp', (n, v))
        assert stop is not None or rst['used'] == len(pieces), (rst['used'], len(pieces))

        sems = {n: es.enter_context(nc.semaphore(n)) for n in P.sem_names()}
        with nc.Block() as block:
            @block.tensor
            def _(e):
                P.replay('pe', e, sems)

            @block.scalar
            def _(e):
                P.replay('act', e, sems)

            @block.vector
            def _(e):
                P.replay('dve', e, sems)

            @block.gpsimd
            def _(e):
                P.replay('pool', e, sems)

            @block.sync
            def _(e):
                P.replay('sp', e, sems)
    return nc


def _feat(v):
    v = np.asarray(v, np.float32)
    lead = v.shape[:-1]
    return np.moveaxis(v.reshape(lead + (8, 128)), -1, 0)


_NC_CACHE = {}


def kernel(x_prompt, x_sample, state_rglru, c, c_ctx, w_mod, b_mod, w_in, conv_a, w_a_out, conv_b, conv_b_bias,
           w_gate_a, b_gate_a, w_gate_x, b_gate_x, lru_lambda, w_b_out, w_o, ln1_g, ln1_b, ln2_g, ln2_b,
           ffn_w1, ffn_w3, ffn_w2, router_w, router_b, moe_w1, moe_w3, moe_w2):
    f32 = lambda a: np.ascontiguousarray(np.asarray(a, np.float32))
    x_prompt, x_sample, state_rglru, c, c_ctx = map(f32, (x_prompt, x_sample, state_rglru, c, c_ctx))
    ncores = 8
    smallp = np.zeros((128, NS), np.float32)
    smallp[:, SP_BMOD:SP_BMOD + 96] = np.moveaxis(f32(b_mod).reshape(L, 48, 128), -1, 0).reshape(128, 96)
    smallp[:, SP_CA:SP_CA + 48] = _feat(conv_a).reshape(128, 48)
    smallp[:, SP_CB:SP_CB + 64] = _feat(conv_b).reshape(128, 64)
    smallp[:, SP_CBB:SP_CBB + 16] = _feat(conv_b_bias).reshape(128, 16)
    smallp[:, SP_BGA:SP_BGA + 32] = _feat(b_gate_a).reshape(128, 32)
    smallp[:, SP_BGX:SP_BGX + 32] = _feat(b_gate_x).reshape(128, 32)
    smallp[:, SP_LAM:SP_LAM + 32] = _feat(lru_lambda).reshape(128, 32)
    smallp[:, SP_RB:SP_RB + 8] = np.broadcast_to(f32(router_b)[0][None, :], (128, 8))
    lnp = np.stack([f32(ln1_g), f32(ln1_b), f32(ln2_g), f32(ln2_b)], axis=1)
    gwbd = np.zeros((L, 4, 8, 128, 128), np.float32)
    wga, wgx = f32(w_gate_a), f32(w_gate_x)
    for d in range(2):
        for g, wsrc in enumerate((wga, wgx)):
            for h in range(16):
                cc, hh = h // 2, h % 2
                gwbd[:, d * 2 + g, cc, hh * 64:(hh + 1) * 64, hh * 64:(hh + 1) * 64] = wsrc[:, d, h]
    ident = np.eye(128, dtype=np.float32)
    shared = dict(smallp=smallp, lnp=lnp, gwbd=gwbd, ident=ident, w_mod=f32(w_mod), w_in=f32(w_in), w_a_out=f32(w_a_out),
                  w_b_out=f32(w_b_out), w_o=f32(w_o), ffn_w1=f32(ffn_w1), ffn_w3=f32(ffn_w3), ffn_w2=f32(ffn_w2),
                  router_w=f32(router_w), moe_w1=f32(moe_w1), moe_w3=f32(moe_w3), moe_w2=f32(moe_w2))
    in_maps = []
    seqmap = []
    for core in range(ncores):
        flags = np.zeros((128, NFL), np.float32)
        h0 = np.zeros((128, L, 2, 8, 5), np.float32)
        cf = np.zeros(20, np.float32)
        if core < 2:
            b = core
            xin = np.concatenate([x_sample[b], x_prompt[b]], axis=0)
            conds = np.stack([c[b], c_ctx], axis=0)
            flags[:, FL_KF + 1:FL_KF + 4] = 1.0
            flags[:, FL_KB + 0:FL_KB + 3] = 1.0
            cf[17:20] = 1.0
            st = _feat(state_rglru[b])
            h0[:, :, 0, :, 0] = st[:, :, 0, :]
            h0[:, :, 1, :, 3] = st[:, :, 1, :]
            seqmap.append([None, None, None, None, b])
        else:
            s0 = 2 + 5 * (core - 2)
            xin = x_prompt[s0:s0 + 5].reshape(NTOK, D)
            conds = np.stack([c_ctx, c_ctx], axis=0)
            for r in range(20):
                cf[r] = 1.0 if r % 4 != 0 else 0.0
            seqmap.append([s0 + i for i in range(5)])
        flags[:, FL_CF:FL_CF + 20] = cf[None, :]
        cond = np.ascontiguousarray(np.moveaxis(conds.reshape(2, 8, 128), -1, 0).transpose(0, 2, 1)).reshape(128, 16)
        m = dict(shared)
        m.update(xin=np.ascontiguousarray(xin), cond=cond, h0=h0.reshape(128, 160), flags=flags)
        in_maps.append(m)
    if 'nc' not in _NC_CACHE:
        _NC_CACHE['nc'] = build_nc()
    res = run_bass_kernel_spmd(_NC_CACHE['nc'], in_maps, core_ids=list(range(ncores)))
    y_prompt = np.zeros((32, 256, D), np.float32)
    y_sample = np.zeros((2, 1024, D), np.float32)
    new_state = np.zeros((32, L, 2, D), np.float32)
    for core in range(ncores):
        y = res.results[core]["yout"]
        st = res.results[core]["stout"].reshape(128, L, 2, 8, 5)
        if core < 2:
            y_sample[core] = y[0:1024]
        for s, seq in enumerate(seqmap[core]):
            if seq is None:
                continue
            y_prompt[seq] = y[s * 256:(s + 1) * 256]
            new_state[seq] = np.moveaxis(st[:, :, :, :, s], 0, -1).reshape(L, 2, D)
    return (y_prompt, y_sample, new_state)
```

```python
import numpy as np
import concourse.bass as bass
import concourse.mybir as mybir
from concourse.bass_utils import run_bass_kernel_spmd

F32 = mybir.dt.float32
BF16 = mybir.dt.bfloat16
AF = mybir.ActivationFunctionType
ALU = mybir.AluOpType
AX = mybir.AxisListType

D = 1024
NTOK = 1280
NTB = 10
NCH = 8
DFF = 2816
NFC = 22
NE = 8
L = 2
NIN = 7168
ALPHA = (2.0 * L) ** 0.25
ENG = ['pe', 'act', 'dve', 'pool', 'sp']
TILES = [(0, 512), (512, 512), (1024, 256)]


class Rec:
    def __init__(self):
        self.call = None

    def __getattr__(self, name):
        def f(*a, **kw):
            self.call = (name, a, kw)
            return self
        return f


class Prog:
    def __init__(self):
        self.q = {e: [] for e in ENG}
        self.cnt = {e: 0 for e in ENG}
        self.waited = {e: {} for e in ENG}
        self.lw = {}
        self.rd = {}
        self.dcnt = {}

    def _wait(self, e, tok):
        name, val = tok
        if name == e and e == 'pe':
            return
        if self.waited[e].get(name, 0) >= val:
            return
        self.waited[e][name] = val
        self.q[e].append(('w', name, val))

    def deps(self, e, reads, writes):
        for k in reads:
            t = self.lw.get(k)
            if t:
                self._wait(e, t)
        for k in writes:
            t = self.lw.get(k)
            if t:
                self._wait(e, t)
            for n, v in self.rd.get(k, {}).items():
                self._wait(e, (n, v))

    def commit(self, tok, reads, writes):
        for k in reads:
            d = self.rd.setdefault(k, {})
            if d.get(tok[0], 0) < tok[1]:
                d[tok[0]] = tok[1]
        for k in writes:
            self.lw[k] = tok
            self.rd[k] = {}

    def op(self, e, fn, reads=(), writes=()):
        psr = [k for k in reads if isinstance(k, tuple) and k[0] == 'ps']
        if psr:
            reads = [k for k in reads if k not in psr]
            writes = list(writes) + psr
        self.deps(e, reads, writes)
        self.cnt[e] += 1
        tok = (e, self.cnt[e])
        rec = Rec()
        fn(rec)
        self.q[e].append(('i', rec.call))
        self.commit(tok, reads, writes)
        return tok

    def dma(self, e, semname, fn, reads=(), writes=()):
        self.deps(e, reads, writes)
        self.dcnt[semname] = self.dcnt.get(semname, 0) + 16
        tok = (semname, self.dcnt[semname])
        rec = Rec()
        fn(rec)
        self.q[e].append(('d', rec.call, semname))
        self.commit(tok, reads, writes)
        return tok

    def wait_tok(self, e, tok):
        self._wait(e, tok)

    def sem_names(self):
        return list(ENG) + sorted(self.dcnt.keys())

    def replay(self, e, eng, sems):
        for it in self.q[e]:
            if it[0] == 'w':
                eng.wait_ge(sems[it[1]], it[2])
            elif it[0] == 'i':
                name, a, kw = it[1]
                getattr(eng, name)(*a, **kw).then_inc(sems[e], 1)
            else:
                name, a, kw = it[1]
                getattr(eng, name)(*a, **kw).then_inc(sems[it[2]], 16)


def mk(ap, off, dims):
    return bass.AP(ap.tensor, ap.offset + off, [list(ap.ap[0])] + [list(d) for d in dims])


SP_BMOD = 0
SP_CA = 96
SP_CB = 144
SP_CBB = 208
SP_BGA = 224
SP_BGX = 256
SP_LAM = 288
SP_RB = 320
NS = 328
FL_KF = 0
FL_KB = 5
FL_CF = 10
NFL = 32
SLOT_BYTES = 12288
NSLOT = 3
QUARTERS = [(0, 6), (6, 6), (12, 6), (18, 4)]


class _Stop(Exception):
    pass


def build_nc(nl=L, stop=None):
    import contextlib
    nc = bass.Bass("TRN2", target_bir_lowering=False)

    def din(name, shape, dt=F32):
        return nc.dram_tensor(name, list(shape), dt, kind="ExternalInput").ap()

    def dout(name, shape, dt=F32):
        return nc.dram_tensor(name, list(shape), dt, kind="ExternalOutput").ap()

    xin = din("xin", [NTOK, D])
    cond_d = din("cond", [128, 16])
    h0_d = din("h0", [128, 160])
    flg_d = din("flags", [128, NFL])
    smallp_d = din("smallp", [128, NS])
    ident_d = din("ident", [128, 128])
    lnp_d = din("lnp", [L, 4, D])
    gwbd_d = din("gwbd", [L, 4, 8, 128, 128])
    w_mod_d = din("w_mod", [L, D, 6 * D])
    w_in_d = din("w_in", [L, D, NIN])
    w_a_out_d = din("w_a_out", [L, D, D])
    w_b_out_d = din("w_b_out", [L, D, D])
    w_o_d = din("w_o", [L, D, D])
    ffn_w1_d = din("ffn_w1", [1, D, DFF])
    ffn_w3_d = din("ffn_w3", [1, D, DFF])
    ffn_w2_d = din("ffn_w2", [1, DFF, D])
    router_w_d = din("router_w", [1, D, NE])
    moe_w1_d = din("moe_w1", [1, NE, D, DFF])
    moe_w3_d = din("moe_w3", [1, NE, D, DFF])
    moe_w2_d = din("moe_w2", [1, NE, DFF, D])
    yout = dout("yout", [NTOK, D])
    stout = dout("stout", [128, 160])

    P = Prog()
    es = contextlib.ExitStack()
    with es:
        def sb(name, n, dt=F32):
            return es.enter_context(nc.sbuf_tensor('s_' + name, [128, n], dt))

        xres_t = sb("xres", NTB * D)
        ubuf_t = sb("ubuf", NCH * NTOK, BF16)
        reg = sb("reg", 79360 // 2, BF16)
        ring_t = sb("ring", NSLOT * SLOT_BYTES // 2, BF16)
        tw = sb("tw", 5 * D)
        gbc = tw[:, 0:D]
        lnbc = tw[:, D:3 * D]
        tmA = tw[:, 3 * D:4 * D]
        xnbf = tw[:, 4 * D:5 * D].bitcast(BF16)
        smallp = sb("smallp", NS)
        flg = sb("flg", NFL)
        h0t = sb("h0t", 160)
        stbuf = sb("stbuf", 160)
        condt = sb("condt", 16)
        scond = sb("scond", 16, BF16)
        identf = sb("identf", 128)
        identb = sb("identb", 128, BF16)
        onesf = sb("onesf", 128)
        diag = sb("diag", 256)
        modT = sb("modT", 96 * L)
        cneg = sb("cneg", 64)
        lamt = sb("lamt", 64)
        wfl = sb("wfl", 100)
        fx = sb("fx", 256)
        mv_t = sb("mv", 2 * 4 * 16)
        rs_t = sb("rs", 2 * 16)
        lnst = {"n": 0, "rs": None}
        epsc = sb("epsc", 4)
        wrb = sb("wrb", 64, BF16)
        lg = sb("lg", 80)
        lg2 = sb("lg2", 80)
        eq1 = sb("eq1", 80)
        eq2 = sb("eq2", 80)
        comb = sb("comb", 80)
        mx = sb("mx", 40)
        ps = es.enter_context(nc.psum_tensor("ps", [128, 8, 512], F32))
        psb = ps[:].bitcast(BF16)

        def cf32(byte_off, n):
            return reg[:, byte_off // 2: byte_off // 2 + 2 * n].bitcast(F32)

        def cbf(byte_off, n):
            return reg[:, byte_off // 2: byte_off // 2 + n]

        xres = xres_t[:].rearrange("p (t f) -> p t f", t=NTB)
        ubuf = ubuf_t[:].rearrange("p (c t) -> p c t", c=NCH)
        a_pre = cbf(0, NCH * NTOK).rearrange("p (c t) -> p c t", c=NCH)
        b_pre = cbf(20480, NCH * NTOK).rearrange("p (c t) -> p c t", c=NCH)
        T = [cf32(40960 + i * 5120, NTOK) for i in range(7)] + [tw[:, i * NTOK:(i + 1) * NTOK] for i in range(3)]
        xrbf = cbf(76800, NTOK)
        merged = cbf(40960, NCH * NTOK).rearrange("p (c t) -> p c t", c=NCH)
        f_acc = cf32(0, NTB * D).rearrange("p (t f) -> p t f", t=NTB)
        actq = [cbf(40960 + i * 15360, 6 * NTOK).rearrange("p (c t) -> p c t", c=6) for i in range(2)]
        tS = [cf32(71680 + i * 2048, 512) for i in range(2)]

        pst = {'ptr': 0, 'reserved': set()}

        def palloc(n=1):
            p = pst['ptr']
            if p % n:
                p += n - p % n
            if p + n > 8:
                p = 0
            while n == 1 and p in pst['reserved']:
                p = (p + 1) % 8
            pst['ptr'] = (p + n) % 8
            return p

        def pk(b, n=1):
            return [('ps', b + i) for i in range(n)]

        pieces = []
        rst = {'issued': 0, 'used': 0}

        def slot_ap(i):
            s = i % NSLOT
            return ring_t[:, s * (SLOT_BYTES // 2):(s + 1) * (SLOT_BYTES // 2)]

        def ring_issue(upto):
            while rst['issued'] < min(upto, len(pieces)):
                i = rst['issued']
                tag, loader = pieces[i]
                s = i % NSLOT
                loader(slot_ap(i), ('ring', s), 'ring%d' % s)
                rst['issued'] += 1

        def ring_next(tag, hold=0):
            i = rst['used']
            assert pieces[i][0] == tag, (pieces[i][0], tag)
            ring_issue(i + NSLOT - hold)
            rst['used'] += 1
            return slot_ap(i), ('ring', i % NSLOT)

        def wdma(dst, src, key, sem):
            P.dma('pool', sem, lambda e: e.dma_start(out=dst, in_=src), writes=[key])

        def kview(w2d):
            return w2d.rearrange("(k p) n -> p k n", p=128)

        def mk_mod_piece(l, n):
            def ld(slot, key, sem):
                dst = slot[:, 0:8 * 512].rearrange("p (k n) -> p k n", k=8)
                wdma(dst, kview(w_mod_d[l])[:, :, n * 512:(n + 1) * 512], key, sem)
            return ld

        def mk_p1_piece(l, c):
            def ld(slot, key, sem):
                wv = kview(w_in_d[l])
                dst = slot[:, 0:5 * 1024].rearrange("p (g k n) -> p g k n", g=5, k=8)
                for gi, grp in enumerate([0, 2, 1, 4, 3]):
                    wdma(dst[:, gi], wv[:, :, grp * D + c * 128: grp * D + (c + 1) * 128], key, sem)
                gdst = slot[:, 5120:5120 + 512].rearrange("p (g n) -> p g n", g=4)
                wdma(gdst, gwbd_d[l, :, c].rearrange("g p n -> p g n"), key, sem)
            return ld

        def mk_p2_piece(l, m):
            def ld(slot, key, sem):
                wv = kview(w_in_d[l])
                dst = slot[:, 0:4 * 1024].rearrange("p (g k n) -> p g k n", g=4, k=8)
                wdma(dst[:, 0], wv[:, :, 5 * D + m * 128: 5 * D + (m + 1) * 128], key, sem)
                wdma(dst[:, 1], wv[:, :, 6 * D + m * 128: 6 * D + (m + 1) * 128], key, sem)
                wdma(dst[:, 2], kview(w_a_out_d[l])[:, :, m * 128:(m + 1) * 128], key, sem)
                wdma(dst[:, 3], kview(w_b_out_d[l])[:, :, m * 128:(m + 1) * 128], key, sem)
            return ld

        def mk_wo_piece(l, h):
            def ld(slot, key, sem):
                dst = slot[:, 0:4 * 1024].rearrange("p (k n) -> p k n", k=4)
                wdma(dst, kview(w_o_d[l])[:, 4 * h:4 * h + 4, :], key, sem)
            return ld

        def mk_f1_piece(w1, w3, f0, nf):
            def ld(slot, key, sem):
                dst = slot[:, 0:2 * 8 * 256].rearrange("p (g k n) -> p g k n", g=2, k=8)
                wdma(dst[:, 0, :, 0:nf * 128], kview(w1)[:, :, f0 * 128:(f0 + nf) * 128], key, sem)
                wdma(dst[:, 1, :, 0:nf * 128], kview(w3)[:, :, f0 * 128:(f0 + nf) * 128], key, sem)
            return ld

        def mk_f2_piece(w2, f0, nf):
            def ld(slot, key, sem):
                dst = slot[:, 0:nf * 1024].rearrange("p (k n) -> p k n", k=nf)
                wdma(dst, kview(w2)[:, f0:f0 + nf, :], key, sem)
            return ld

        def experts(l):
            if l % 2 == 0:
                return [(ffn_w1_d[l // 2], ffn_w3_d[l // 2], ffn_w2_d[l // 2])]
            return [(moe_w1_d[l // 2, e], moe_w3_d[l // 2, e], moe_w2_d[l // 2, e]) for e in range(NE)]

        def mod_jobs_at(site):
            kind, l = site[0], site[1]
            if kind == 'p1':
                jobs = [(l, 4 + site[2])]
                if site[2] == NCH - 1 and l + 1 < nl:
                    jobs += [(l + 1, n) for n in range(4)]
                return jobs
            return []

        for n in range(4):
            pieces.append((('mod', 0, n), mk_mod_piece(0, n)))
        for l in range(nl):
            for c in range(NCH):
                pieces.append((('p1', l, c), mk_p1_piece(l, c)))
                for (ml, n) in mod_jobs_at(('p1', l, c)):
                    pieces.append((('mod', ml, n), mk_mod_piece(ml, n)))
            for m in range(NCH):
                pieces.append((('p2', l, m), mk_p2_piece(l, m)))
                for (ml, n) in mod_jobs_at(('p2', l, m)):
                    pieces.append((('mod', ml, n), mk_mod_piece(ml, n)))
            for h in range(2):
                pieces.append((('wo', l, h), mk_wo_piece(l, h)))
            for e, (w1, w3, w2) in enumerate(experts(l)):
                for q, (f0, nf) in enumerate(QUARTERS):
                    for i in range(0, nf, 2):
                        pieces.append((('f1', l, e, q, i), mk_f1_piece(w1, w3, f0 + i, 2)))
                        for (ml, n) in mod_jobs_at(('f1', l, e, q, i)):
                            pieces.append((('mod', ml, n), mk_mod_piece(ml, n)))
                    pieces.append((('f2', l, e, q), mk_f2_piece(w2, f0, nf)))

        def spc(col, n=1):
            return smallp[:, col:col + n]

        def V(e):
            return e

        def dve(fn, r=(), w=()):
            return P.op('dve', fn, r, w)

        def act(fn, r=(), w=()):
            return P.op('act', fn, r, w)

        def pool(fn, r=(), w=()):
            return P.op('pool', fn, r, w)

        def pe(fn, r=(), w=()):
            return P.op('pe', fn, r, w)

        def kt(name):
            return [(name, j) for j in range(3)]

        xin_v = xin.rearrange("(t p) f -> p t f", p=128)
        for j, (t0, tn) in enumerate(TILES):
            a, b = t0 // 128, (t0 + tn) // 128
            P.dma('sp', 'ldx%d' % j, lambda e: e.dma_start(out=xres[:, a:b, :], in_=xin_v[:, a:b, :]), writes=[('x', tb) for tb in range(a, b)])
        for ii, (dst, src, key) in enumerate([(smallp, smallp_d, 'smallp'), (flg, flg_d, 'flg'), (h0t, h0_d, 'h0'),
                                              (condt, cond_d, 'cond'), (identf, ident_d, 'identf')]):
            P.dma('sp', 'lds%d' % ii, lambda e, dst=dst, src=src: e.dma_start(out=dst[:], in_=src), writes=[key])
        wdma(wrb[:].rearrange("p (k n) -> p k n", k=8), kview(router_w_d[0]), 'wrb', 'ldr')
        act(lambda e: e.activation(out=scond[:], in_=condt[:], func=AF.Silu), ['cond'], ['scond'])
        dve(lambda e: e.tensor_copy(out=identb[:], in_=identf[:]), ['identf'], ['identb'])
        dve(lambda e: e.memset(onesf[:], 1.0), [], ['onesf'])
        dve(lambda e: e.memset(stbuf[:], 0.0), [], ['stbuf'])
        dve(lambda e: e.memset(epsc[:, 0:1], 1e-6), [], ['epsc'])
        dve(lambda e: e.memset(epsc[:, 1:2], 1e-5), [], ['epsc'])
        dve(lambda e: e.memset(epsc[:, 2:3], 1.0), [], ['epsc'])
        act(lambda e: e.activation(out=lamt[:, 0:32], in_=spc(SP_LAM, 32), func=AF.Exp, scale=-1.0), ['smallp'], ['lamt'])
        act(lambda e: e.activation(out=lamt[:, 32:64], in_=lamt[:, 0:32], func=AF.Ln, bias=epsc[:, 2:3], scale=1.0), ['lamt', 'epsc'], ['lamt'])
        dve(lambda e: e.tensor_scalar(out=cneg[:, 0:32], in0=lamt[:, 32:64], scalar1=-8.0, scalar2=None, op0=ALU.mult), ['lamt'], ['cneg'])
        dve(lambda e: e.tensor_scalar(out=cneg[:, 32:64], in0=lamt[:, 32:64], scalar1=-16.0, scalar2=None, op0=ALU.mult), ['lamt'], ['cneg'])

        out_toks = []

        def ln_stats_tile(tbs, eps_col, src_keyf):
            n = len(tbs)
            par = lnst["n"] % 2
            lnst["n"] += 1
            mv = mv_t[:, par * 64:(par + 1) * 64]
            rs = rs_t[:, par * 16:(par + 1) * 16]
            rsk = ("rs", par)
            lnst["rs"] = (rs, rsk)
            for i, tb in enumerate(tbs):
                o = i * 16
                dve(lambda e, tb=tb, o=o: e.bn_stats(out=mv[:, o:o + 6], in_=xres[:, tb, 0:512]), [src_keyf(tb)], [('mv', par, i)])
                dve(lambda e, tb=tb, o=o: e.bn_stats(out=mv[:, o + 6:o + 12], in_=xres[:, tb, 512:1024]), [src_keyf(tb)], [('mv', par, i)])
                dve(lambda e, o=o: e.bn_aggr(out=mv[:, o + 12:o + 14], in_=mv[:, o:o + 12]), [('mv', par, i)], [('mv', par, i)])
            mvv = mv.rearrange("p (i s) -> p i s", s=16)
            mvk = [('mv', par, i) for i in range(n)]
            act(lambda e: e.activation(out=rs[:, 0:n], in_=mvv[:, 0:n, 13], func=AF.Sqrt, bias=epsc[:, eps_col:eps_col + 1], scale=1.0),
                mvk + ['epsc'], [rsk])
            dve(lambda e: e.reciprocal(out=rs[:, 4:4 + n], in_=rs[:, 0:n]), [rsk], [rsk])
            dve(lambda e: e.scalar_tensor_tensor(out=rs[:, 8:8 + n], in0=mvv[:, 0:n, 12], scalar=-1.0, in1=rs[:, 4:4 + n],
                                                 op0=ALU.mult, op1=ALU.mult), mvk + [rsk], [rsk])

        def build_u(l, j, shc, scc, src_keyf):
            t0, tn = TILES[j]
            tbs = list(range(t0 // 128, (t0 + tn) // 128))
            slot = 0 if j < 2 else 1
            ln_stats_tile(tbs, 0, src_keyf)
            rs, rsk = lnst["rs"]
            b0 = palloc(4)
            for i, tb in enumerate(tbs):
                xb = xnbf[:, (i % 2) * D:(i % 2 + 1) * D]
                act(lambda e, xb=xb, tb=tb, i=i: e.activation(out=xb, in_=xres[:, tb, :], func=AF.Identity,
                                                             scale=rs[:, 4 + i:5 + i], bias=rs[:, 8 + i:9 + i]),
                    [src_keyf(tb), rsk], [('xnbf', i % 2)])
                for c in range(NCH):
                    pe(lambda e, xb=xb, c=c, i=i: e.transpose(out=psb[:, b0 + i, c * 128:(c + 1) * 128], in_=xb[:, c * 128:(c + 1) * 128],
                                                           identity=identb[:]),
                       [('xnbf', i % 2), 'identb'], pk(b0 + i))
            nb = len(tbs)
            hb = nb // 2
            for c in range(NCH):
                s1 = modT[:, l * 96 + (scc + c) * 2 + slot:l * 96 + (scc + c) * 2 + slot + 1]
                s2 = modT[:, l * 96 + (shc + c) * 2 + slot:l * 96 + (shc + c) * 2 + slot + 1]
                src = psb[:, b0:b0 + hb, c * 128:(c + 1) * 128]
                dst = ubuf[:, c, t0:t0 + hb * 128].rearrange("p (a b) -> p a b", a=hb)
                dve(lambda e, src=src, dst=dst, s1=s1, s2=s2: e.tensor_scalar(out=dst, in0=src, scalar1=s1, scalar2=s2,
                                                                            op0=ALU.mult, op1=ALU.add),
                    pk(b0, hb) + [('modT', l)], [('u', c, j, 0)])
                src = psb[:, b0 + hb:b0 + nb, c * 128:(c + 1) * 128]
                dst = ubuf[:, c, t0 + hb * 128:t0 + tn].rearrange("p (a b) -> p a b", a=nb - hb)
                act(lambda e, src=src, dst=dst, s1=s1, s2=s2: e.activation(out=dst, in_=src, func=AF.Identity, scale=s1, bias=s2),
                    pk(b0 + hb, nb - hb) + [('modT', l)], [('u', c, j, 1)])

        def build_gbc(l, chunk0, slot):
            b = palloc(2)
            for c in range(NCH):
                dg = diag[:, (c % 2) * 128:(c % 2 + 1) * 128]
                col = modT[:, l * 96 + (chunk0 + c) * 2 + slot:l * 96 + (chunk0 + c) * 2 + slot + 1]
                dve(lambda e, dg=dg, col=col: e.tensor_scalar(out=dg, in0=identf[:], scalar1=col, scalar2=None, op0=ALU.mult),
                    ['identf', ('modT', l)], [('diag', c % 2)])
                pe(lambda e, dg=dg, c=c: e.matmul(ps[:, b + c // 4, (c % 4) * 128:(c % 4 + 1) * 128], lhsT=onesf[:], rhs=dg, start=True, stop=True),
                   ['onesf', ('diag', c % 2)], pk(b + c // 4))
            act(lambda e: e.activation(out=gbc.rearrange("p (a b) -> p a b", a=2), in_=ps[:, b:b + 2, :], func=AF.Copy),
                pk(b, 2), ['gbc'])

        def load_lnbc(l, i0):
            for i in range(2):
                src = bass.AP(lnp_d.tensor, lnp_d[l, i0 + i, :].offset, [[0, 128], [1, D]])
                P.dma('sp', 'ldln%d' % i, lambda e, src=src, i=i: e.dma_start(out=lnbc[:, i * D:(i + 1) * D], in_=src), writes=[('lnbc', i)])

        def resid_ln(l, tbs, pkeys_f, src_ap_f, eps_col, last):
            for tb in tbs:
                dve(lambda e, tb=tb: e.tensor_tensor(out=tmA, in0=src_ap_f(tb), in1=gbc, op=ALU.mult),
                    pkeys_f(tb) + ['gbc'], ['tmA'])
                dve(lambda e, tb=tb: e.scalar_tensor_tensor(out=xres[:, tb, :], in0=xres[:, tb, :], scalar=float(ALPHA), in1=tmA,
                                                           op0=ALU.mult, op1=ALU.add), ['tmA', ('x', tb)], [('x', tb)])
            ln_stats_tile(tbs, eps_col, lambda tb: ('x', tb))
            rs, rsk = lnst["rs"]
            for i, tb in enumerate(tbs):
                act(lambda e, tb=tb, i=i: e.activation(out=xres[:, tb, :], in_=xres[:, tb, :], func=AF.Identity,
                                                       scale=rs[:, 4 + i:5 + i], bias=rs[:, 8 + i:9 + i]), [('x', tb), rsk], [('x', tb)])
                dve(lambda e, tb=tb: e.tensor_tensor(out=xres[:, tb, :], in0=xres[:, tb, :], in1=lnbc[:, 0:D], op=ALU.mult),
                    [('x', tb), ('lnbc', 0)], [('x', tb)])
                pool(lambda e, tb=tb: e.tensor_tensor(out=xres[:, tb, :], in0=xres[:, tb, :], in1=lnbc[:, D:2 * D], op=ALU.add),
                     [('x', tb), ('lnbc', 1)], [('x', tb)])
                if last:
                    tk = P.dma('sp', 'sty', lambda e, tb=tb: e.dma_start(out=yout[tb * 128:(tb + 1) * 128, :], in_=xres[:, tb, :]),
                               reads=[('x', tb)])
                    out_toks.append(tk)

        TWK = ['gbc', ('lnbc', 0), ('lnbc', 1), 'tmA']
        T79K = kt('T7') + kt('T8') + kt('T9')

        def barrier(keys, engines=('act', 'dve', 'pool', 'sp')):
            for en in engines:
                P.deps(en, [], keys)

        def chk(tag):
            if stop == tag:
                raise _Stop()

        def mod_job(ml, n):
            bank = palloc(1)
            slot, skey = ring_next(('mod', ml, n))
            wv = slot[:, 0:8 * 512].rearrange("p (k n) -> p k n", k=8)
            for f4 in range(4):
                for k in range(8):
                    pe(lambda e, wv=wv, f4=f4, k=k: e.matmul(ps[:, bank, f4 * 2:f4 * 2 + 2], lhsT=wv[:, k, f4 * 128:(f4 + 1) * 128],
                                                          rhs=scond[:, k * 2:k * 2 + 2], start=(k == 0), stop=(k == 7)),
                       [skey, 'scond'], pk(bank))
            c_lo = n * 4
            for s_ in range(2):
                dve(lambda e, s_=s_: e.tensor_tensor(out=mk(modT[:, 0:1], ml * 96 + c_lo * 2 + s_, [[2, 4]]),
                                                    in0=mk(ps[:, bank, 0:1], s_, [[2, 4]]),
                                                    in1=spc(SP_BMOD + ml * 48 + c_lo, 4), op=ALU.add), pk(bank) + ['smallp'], [('modT', ml)])
            if n in (2, 3, 8, 9):
                dve(lambda e: e.tensor_scalar(out=modT[:, ml * 96 + c_lo * 2:ml * 96 + c_lo * 2 + 8], in0=modT[:, ml * 96 + c_lo * 2:ml * 96 + c_lo * 2 + 8],
                                              scalar1=1.0, scalar2=None, op0=ALU.add), [('modT', ml)], [('modT', ml)])

        for n in range(4):
            mod_job(0, n)

        try:
            for l in range(nl):
                chk('mod')
                for j in range(3):
                    build_u(l, j, 0, 8, lambda tb: ('x', tb))
                chk('u')

                barrier(TWK, ('act', 'dve', 'pool'))
                for c in range(NCH):
                    slot, skey = ring_next(('p1', l, c))
                    wv = slot[:, 0:5 * 1024].rearrange("p (g k n) -> p g k n", g=5, k=8)
                    gwv = slot[:, 5120:5120 + 512].rearrange("p (g n) -> p g n", g=4)

                    def proj(gi):
                        banks = []
                        for j, (t0, tn) in enumerate(TILES):
                            b = palloc(1)
                            for k in range(8):
                                pe(lambda e, b=b, gi=gi, k=k, t0=t0, tn=tn: e.matmul(ps[:, b, 0:tn], lhsT=wv[:, gi, k, :], rhs=ubuf[:, k, t0:t0 + tn],
                                                                                 start=(k == 0), stop=(k == 7)),
                                   [skey, ('u', k, j, 0), ('u', k, j, 1)], pk(b))
                            banks.append(b)
                        return banks

                    def rows(t, lo, hi, r0=0, r1=20):
                        return mk(t[:, 0:1], r0 * 64 + lo, [[64, r1 - r0], [1, hi - lo]])

                    wa = [spc(SP_CA + (l * 3 + jj) * 8 + c) for jj in range(3)]
                    wb = [spc(SP_CB + (l * 4 + jj) * 8 + c) for jj in range(4)]
                    bx = proj(0)
                    for j, (t0, tn) in enumerate(TILES):
                        act(lambda e, j=j, t0=t0, tn=tn: e.activation(out=T[0][:, t0:t0 + tn], in_=ps[:, bx[j], 0:tn], func=AF.Copy),
                            pk(bx[j]), [('T0', j)])
                    bc_ = proj(1)
                    for j, (t0, tn) in enumerate(TILES):
                        dve(lambda e, j=j, t0=t0, tn=tn: e.tensor_tensor(out=T[1][:, t0:t0 + tn], in0=ps[:, bc_[j], 0:tn], in1=T[0][:, t0:t0 + tn],
                                                                      op=ALU.mult), pk(bc_[j]) + [('T0', j)], [('T1', j)])
                    for i, wcol in enumerate([wa[0], wa[2]]):
                        dve(lambda e, i=i, wcol=wcol: e.tensor_scalar(out=wfl[:, i * 20:(i + 1) * 20], in0=flg[:, FL_CF:FL_CF + 20], scalar1=wcol,
                                                                      scalar2=None, op0=ALU.mult), ['flg', 'smallp'], ['wfla'])
                    p_, acc = T[1], T[2]
                    dve(lambda e: e.tensor_scalar(out=acc[:, :], in0=p_[:, :], scalar1=wa[1], scalar2=None, op0=ALU.mult), kt('T1') + ['smallp'], kt('T2'))
                    dve(lambda e: e.scalar_tensor_tensor(out=rows(acc, 1, 64), in0=rows(p_, 0, 63), scalar=wa[0], in1=rows(acc, 1, 64),
                                                         op0=ALU.mult, op1=ALU.add), kt('T1') + kt('T2') + ['smallp'], kt('T2'))
                    dve(lambda e: e.scalar_tensor_tensor(out=rows(acc, 0, 63), in0=rows(p_, 1, 64), scalar=wa[2], in1=rows(acc, 0, 63),
                                                         op0=ALU.mult, op1=ALU.add), kt('T1') + kt('T2') + ['smallp'], kt('T2'))
                    dve(lambda e: e.tensor_tensor(out=fx[:, 0:19], in0=mk(p_[:, 0:1], 63, [[64, 19]]), in1=wfl[:, 1:20], op=ALU.mult),
                         kt('T1') + ['wfla'], ['fxa'])
                    dve(lambda e: e.tensor_tensor(out=mk(acc[:, 0:1], 64, [[64, 19]]), in0=mk(acc[:, 0:1], 64, [[64, 19]]), in1=fx[:, 0:19], op=ALU.add),
                         kt('T2') + ['fxa'], kt('T2'))
                    dve(lambda e: e.tensor_tensor(out=fx[:, 32:51], in0=mk(p_[:, 0:1], 64, [[64, 19]]), in1=wfl[:, 21:40], op=ALU.mult),
                         kt('T1') + ['wfla'], ['fxa'])
                    dve(lambda e: e.tensor_tensor(out=mk(acc[:, 0:1], 63, [[64, 19]]), in0=mk(acc[:, 0:1], 63, [[64, 19]]), in1=fx[:, 32:51], op=ALU.add),
                         kt('T2') + ['fxa'], kt('T2'))
                    br_ = proj(3)
                    for j, (t0, tn) in enumerate(TILES):
                        act(lambda e, j=j, t0=t0, tn=tn: e.activation(out=T[3][:, t0:t0 + tn], in_=ps[:, br_[j], 0:tn], func=AF.Copy),
                            pk(br_[j]), [('T3', j)])
                    for i, wcol in enumerate([wb[0], wb[1], wb[3]]):
                        dve(lambda e, i=i, wcol=wcol: e.tensor_scalar(out=wfl[:, (i + 2) * 20:(i + 3) * 20], in0=flg[:, FL_CF:FL_CF + 20], scalar1=wcol,
                                                                     scalar2=None, op0=ALU.mult), ['flg', 'smallp'], ['wflb'])
                    xq, xr = T[3], T[4]
                    dve(lambda e: e.tensor_scalar(out=xr[:, :], in0=xq[:, :], scalar1=wb[2], scalar2=spc(SP_CBB + l * 8 + c), op0=ALU.mult, op1=ALU.add),
                        kt('T3') + ['smallp'], kt('T4'))
                    for (sh, wcol) in [(2, wb[0]), (1, wb[1])]:
                        dve(lambda e, sh=sh, wcol=wcol: e.scalar_tensor_tensor(out=rows(xr, sh, 64), in0=rows(xq, 0, 64 - sh), scalar=wcol,
                                                                             in1=rows(xr, sh, 64), op0=ALU.mult, op1=ALU.add),
                            kt('T3') + kt('T4') + ['smallp'], kt('T4'))
                    dve(lambda e: e.scalar_tensor_tensor(out=rows(xr, 0, 63), in0=rows(xq, 1, 64), scalar=wb[3], in1=rows(xr, 0, 63),
                                                         op0=ALU.mult, op1=ALU.add), kt('T3') + kt('T4') + ['smallp'], kt('T4'))
                    dve(lambda e: e.tensor_tensor(out=fx[:, 64:102].rearrange("p (a b) -> p a b", b=2), in0=mk(xq[:, 0:1], 62, [[64, 19], [1, 2]]),
                                                  in1=mk(wfl[:, 0:1], 41, [[1, 19], [0, 2]]), op=ALU.mult), kt('T3') + ['wflb'], ['fxb'])
                    dve(lambda e: e.tensor_tensor(out=mk(xr[:, 0:1], 64, [[64, 19], [1, 2]]), in0=mk(xr[:, 0:1], 64, [[64, 19], [1, 2]]),
                                                  in1=fx[:, 64:102].rearrange("p (a b) -> p a b", b=2), op=ALU.add), kt('T4') + ['fxb'], kt('T4'))
                    dve(lambda e: e.tensor_tensor(out=fx[:, 128:147], in0=mk(xq[:, 0:1], 63, [[64, 19]]), in1=wfl[:, 61:80], op=ALU.mult),
                        kt('T3') + ['wflb'], ['fxb'])
                    dve(lambda e: e.tensor_tensor(out=mk(xr[:, 0:1], 64, [[64, 19]]), in0=mk(xr[:, 0:1], 64, [[64, 19]]), in1=fx[:, 128:147], op=ALU.add),
                        kt('T4') + ['fxb'], kt('T4'))
                    dve(lambda e: e.tensor_tensor(out=fx[:, 160:179], in0=mk(xq[:, 0:1], 64, [[64, 19]]), in1=wfl[:, 81:100], op=ALU.mult),
                        kt('T3') + ['wflb'], ['fxb'])
                    dve(lambda e: e.tensor_tensor(out=mk(xr[:, 0:1], 63, [[64, 19]]), in0=mk(xr[:, 0:1], 63, [[64, 19]]), in1=fx[:, 160:179], op=ALU.add),
                        kt('T4') + ['fxb'], kt('T4'))
                    act(lambda e: e.activation(out=xrbf[:, :], in_=xr[:, :], func=AF.Copy), kt('T4'), kt('xrbf'))
                    bb = proj(2)
                    for j, (t0, tn) in enumerate(TILES):
                        dve(lambda e, j=j, t0=t0, tn=tn: e.tensor_tensor(out=a_pre[:, c, t0:t0 + tn], in0=ps[:, bb[j], 0:tn], in1=acc[:, t0:t0 + tn],
                                                                      op=ALU.mult), pk(bb[j]) + [('T2', j)], [('apre', c, j)])
                    by = proj(4)
                    pst['reserved'] = set(by)
                    RT, IT, AT = (T[0], T[7]), (T[1], T[8]), (T[2], T[9])
                    RK, IK, AK = ('T0', 'T7'), ('T1', 'T8'), ('T2', 'T9')
                    HT, HK = (T[5], T[6]), ('T5', 'T6')
                    for d in range(2):
                        for g, dstT, dk, bcol in [(0, RT[d], RK[d], SP_BGA), (1, IT[d], IK[d], SP_BGX)]:
                            for j, (t0, tn) in enumerate(TILES):
                                b = palloc(1)
                                pe(lambda e, b=b, g=g, d=d, t0=t0, tn=tn: e.matmul(ps[:, b, 0:tn], lhsT=gwv[:, d * 2 + g, :], rhs=xrbf[:, t0:t0 + tn],
                                                                               start=True, stop=True), [skey, ('xrbf', j)], pk(b))
                                act(lambda e, b=b, dstT=dstT, t0=t0, tn=tn, bcol=bcol, d=d: e.activation(
                                    out=dstT[:, t0:t0 + tn], in_=ps[:, b, 0:tn], func=AF.Sigmoid,
                                    bias=spc(bcol + (l * 2 + d) * 8 + c), scale=1.0), pk(b) + ['smallp'], [(dk, j)])
                    for d in range(2):
                        cn = cneg[:, (l * 2 + d) * 8 + c:(l * 2 + d) * 8 + c + 1]
                        act(lambda e, d=d, cn=cn: e.activation(out=AT[d][:, :], in_=RT[d][:, :], func=AF.Exp, scale=cn), kt(RK[d]) + ['cneg'], kt(AK[d]))
                    for d in range(2):
                        dve(lambda e, d=d: e.tensor_tensor(out=RT[d][:, :], in0=AT[d][:, :], in1=AT[d][:, :], op=ALU.mult), kt(AK[d]), kt(RK[d]))
                    for d in range(2):
                        dve(lambda e, d=d: e.tensor_tensor(out=IT[d][:, :], in0=IT[d][:, :], in1=xr[:, :], op=ALU.mult), kt(IK[d]) + kt('T4'), kt(IK[d]))
                    for d in range(2):
                        act(lambda e, d=d: e.activation(out=RT[d][:, :], in_=RT[d][:, :], func=AF.Sqrt, scale=-1.0, bias=epsc[:, 2:3]),
                            kt(RK[d]) + ['epsc'], kt(RK[d]))
                    for j, (t0, tn) in enumerate(TILES):
                        act(lambda e, j=j, t0=t0, tn=tn: e.activation(out=T[3][:, t0:t0 + tn], in_=ps[:, by[j], 0:tn], func=AF.Gelu_apprx_tanh),
                            pk(by[j]), [('T3', j)])
                    pst['reserved'] = set()
                    for d in range(2):
                        aT, iT, hT = AT[d], IT[d], HT[d]
                        dve(lambda e, d=d: e.tensor_tensor(out=IT[d][:, :], in0=IT[d][:, :], in1=RT[d][:, :], op=ALU.mult), kt(IK[d]) + kt(RK[d]), kt(IK[d]))
                        col0 = 0 if d == 0 else 255
                        acols = mk(aT[:, 0:1], col0, [[256, 5]])
                        bcols = mk(iT[:, 0:1], col0, [[256, 5]])
                        h0v = h0t[:, ((l * 2 + d) * 8 + c) * 5:((l * 2 + d) * 8 + c) * 5 + 5]
                        kf = flg[:, (FL_KF if d == 0 else FL_KB):(FL_KF if d == 0 else FL_KB) + 5]
                        fo = 200 + d * 8
                        dve(lambda e, fo=fo: e.tensor_tensor(out=fx[:, fo:fo + 5], in0=acols, in1=h0v, op=ALU.mult), kt(AK[d]) + ['h0'], [('fxs', d)])
                        dve(lambda e, fo=fo: e.tensor_tensor(out=bcols, in0=bcols, in1=fx[:, fo:fo + 5], op=ALU.add), kt(IK[d]) + [('fxs', d)], kt(IK[d]))
                        dve(lambda e: e.tensor_tensor(out=acols, in0=acols, in1=kf, op=ALU.mult), kt(AK[d]) + ['flg'], kt(AK[d]))
                        if d == 0:
                            dve(lambda e: e.tensor_tensor_scan(out=hT[:, :], data0=aT[:, :], data1=iT[:, :], initial=0.0, op0=ALU.mult, op1=ALU.add),
                                kt(IK[d]) + kt(AK[d]), kt(HK[d]))
                        else:
                            rv = lambda t: mk(t[:, 0:1], NTOK - 1, [[-1, NTOK]])
                            dve(lambda e: e.tensor_tensor_scan(out=rv(hT), data0=rv(aT), data1=rv(iT), initial=0.0, op0=ALU.mult, op1=ALU.add),
                                kt(IK[d]) + kt(AK[d]), kt(HK[d]))
                        so = ((l * 2 + d) * 8 + c) * 5
                        act(lambda e, so=so, d=d: e.activation(out=stbuf[:, so:so + 5], in_=mk(HT[d][:, 0:1], 255 if d == 0 else 0, [[256, 5]]), func=AF.Copy),
                            kt(HK[d]), ['stbuf'])
                    dve(lambda e: e.tensor_tensor(out=T[5][:, :], in0=T[5][:, :], in1=T[6][:, :], op=ALU.add), kt('T5') + kt('T6'), kt('T5'))
                    dve(lambda e: e.tensor_tensor(out=b_pre[:, c, :], in0=T[5][:, :], in1=T[3][:, :], op=ALU.mult),
                         kt('T5') + kt('T3'), [('bpre', c, j) for j in range(3)])
                    for (ml, n) in mod_jobs_at(('p1', l, c)):
                        mod_job(ml, n)

                barrier(T79K)
                chk('p1')
                for m in range(NCH):
                    slot, skey = ring_next(('p2', l, m))
                    wv = slot[:, 0:4 * 1024].rearrange("p (g k n) -> p g k n", g=4, k=8)
                    for j, (t0, tn) in enumerate(TILES):
                        bs = []
                        for gi in range(4):
                            b = palloc(1)
                            src, skn = (ubuf, 'u') if gi < 2 else ((a_pre, 'apre') if gi == 2 else (b_pre, 'bpre'))
                            for k in range(8):
                                pe(lambda e, b=b, gi=gi, k=k, src=src, t0=t0, tn=tn: e.matmul(ps[:, b, 0:tn], lhsT=wv[:, gi, k, :], rhs=src[:, k, t0:t0 + tn],
                                                                                          start=(k == 0), stop=(k == 7)), [skey] + ([('u', k, j, 0), ('u', k, j, 1)] if skn == 'u' else [(skn, k, j)]), pk(b))
                            bs.append(b)
                        sa, sb_ = T[4][:, t0:t0 + tn], T[5][:, t0:t0 + tn]
                        act(lambda e, b=bs[0], sa=sa, tn=tn: e.activation(out=sa, in_=ps[:, b, 0:tn], func=AF.Sigmoid), pk(bs[0]), [('T4', j)])
                        act(lambda e, b=bs[1], sb_=sb_, tn=tn: e.activation(out=sb_, in_=ps[:, b, 0:tn], func=AF.Sigmoid), pk(bs[1]), [('T5', j)])
                        dve(lambda e, b=bs[2], sa=sa, tn=tn: e.tensor_tensor(out=sa, in0=ps[:, b, 0:tn], in1=sa, op=ALU.mult), pk(bs[2]) + [('T4', j)], [('T4', j)])
                        dve(lambda e, b=bs[3], sb_=sb_, tn=tn: e.tensor_tensor(out=sb_, in0=ps[:, b, 0:tn], in1=sb_, op=ALU.mult), pk(bs[3]) + [('T5', j)], [('T5', j)])
                        dve(lambda e, sa=sa, sb_=sb_, t0=t0, tn=tn: e.tensor_tensor(out=merged[:, m, t0:t0 + tn], in0=sa, in1=sb_, op=ALU.add),
                            [('T4', j), ('T5', j)], [('mg', m, j)])
                    for (ml, n) in mod_jobs_at(('p2', l, m)):
                        mod_job(ml, n)

                chk('p2')
                load_lnbc(l, 0)
                wo = []
                for h in range(2):
                    slot, skey = ring_next(('wo', l, h), hold=h)
                    wo.append((slot[:, 0:4 * 1024].rearrange("p (k n) -> p k n", k=4), skey))
                for j, (t0, tn) in enumerate(TILES):
                    tbs = list(range(t0 // 128, (t0 + tn) // 128))
                    if j == 0 or j == 2:
                        build_gbc(l, 16, 0 if j == 0 else 1)
                    pb = {}
                    for tb in tbs:
                        b = palloc(2)
                        pb[tb] = b
                        for k in range(8):
                            wv, skey = wo[k // 4]
                            for hf in range(2):
                                pe(lambda e, b=b, k=k, hf=hf, tb=tb, wv=wv: e.matmul(ps[:, b + hf, :], lhsT=merged[:, k, tb * 128:(tb + 1) * 128],
                                                                                  rhs=wv[:, k % 4, hf * 512:(hf + 1) * 512], start=(k == 0), stop=(k == 7)),
                                   [skey, ('mg', k, j)], pk(b + hf))
                    resid_ln(l, tbs, lambda tb: pk(pb[tb], 2), lambda tb: ps[:, pb[tb]:pb[tb] + 2, :].rearrange("p a b -> p (a b)"), 1, False)
                    build_u(l, j, 24, 32, lambda tb: ('x', tb))

                chk('p3')
                load_lnbc(l, 2)
                exps = experts(l)
                moe = len(exps) > 1
                if moe:
                    bl = palloc(1)
                    wrv = wrb[:].rearrange("p (k n) -> p k n", k=8)
                    for tb in range(NTB):
                        for k in range(8):
                            pe(lambda e, tb=tb, k=k: e.matmul(ps[:, bl, tb * 8:(tb + 1) * 8], lhsT=ubuf[:, k, tb * 128:(tb + 1) * 128], rhs=wrv[:, k, :],
                                                              start=(k == 0), stop=(k == 7)), ['wrb', ('u', k, tb // 4, 0), ('u', k, tb // 4, 1)], pk(bl))
                    v3 = lambda t: t[:].rearrange("p (a b) -> p a b", b=8)
                    bc3 = lambda t, o=0: mk(t[:, 0:1], o, [[1, 10], [0, 8]])
                    dve(lambda e: e.tensor_tensor(out=v3(lg), in0=ps[:, bl, 0:80].rearrange("p (a b) -> p a b", b=8),
                                                  in1=mk(smallp[:, 0:1], SP_RB, [[0, 10], [1, 8]]), op=ALU.add), pk(bl) + ['smallp'], ['lg'])
                    dve(lambda e: e.tensor_reduce(out=mx[:, 0:10], in_=v3(lg), axis=AX.X, op=ALU.max), ['lg'], ['mx'])
                    dve(lambda e: e.tensor_tensor(out=v3(eq1), in0=v3(lg), in1=bc3(mx, 0), op=ALU.is_equal), ['lg', 'mx'], ['eq1'])
                    dve(lambda e: e.scalar_tensor_tensor(out=lg2[:], in0=eq1[:], scalar=-1e30, in1=lg[:], op0=ALU.mult, op1=ALU.add), ['eq1', 'lg'], ['lg2'])
                    dve(lambda e: e.tensor_reduce(out=mx[:, 10:20], in_=v3(lg2), axis=AX.X, op=ALU.max), ['lg2'], ['mx'])
                    dve(lambda e: e.tensor_tensor(out=v3(eq2), in0=v3(lg2), in1=bc3(mx, 10), op=ALU.is_equal), ['lg2', 'mx'], ['eq2'])
                    dve(lambda e: e.tensor_tensor(out=mx[:, 20:30], in0=mx[:, 0:10], in1=mx[:, 10:20], op=ALU.subtract), ['mx'], ['mx'])
                    act(lambda e: e.activation(out=mx[:, 30:40], in_=mx[:, 20:30], func=AF.Sigmoid, scale=-1.0), ['mx'], ['mx'])
                    act(lambda e: e.activation(out=mx[:, 20:30], in_=mx[:, 20:30], func=AF.Sigmoid), ['mx'], ['mx'])
                    dve(lambda e: e.tensor_tensor(out=v3(eq1), in0=v3(eq1), in1=bc3(mx, 20), op=ALU.mult), ['eq1', 'mx'], ['eq1'])
                    dve(lambda e: e.tensor_tensor(out=v3(eq2), in0=v3(eq2), in1=bc3(mx, 30), op=ALU.mult), ['eq2', 'mx'], ['eq2'])
                    dve(lambda e: e.tensor_tensor(out=comb[:], in0=eq1[:], in1=eq2[:], op=ALU.add), ['eq1', 'eq2'], ['comb'])
                first = True
                qi = 0

                def final_tile(j):
                    t0, tn = TILES[j]
                    tbs = list(range(t0 // 128, (t0 + tn) // 128))
                    if j == 0 or j == 2:
                        build_gbc(l, 40, 0 if j == 0 else 1)
                    resid_ln(l, tbs, lambda tb: [('facc', tb)], lambda tb: f_acc[:, tb, :], 1, l == nl - 1)

                for ei, (w1, w3, w2) in enumerate(exps):
                    for q, (f0, nf) in enumerate(QUARTERS):
                        lastq = (ei == len(exps) - 1) and (q == len(QUARTERS) - 1)
                        aq = actq[qi % 2]
                        aqk = 'aq%d' % (qi % 2)
                        qi += 1
                        for i in range(0, nf, 2):
                            slot, skey = ring_next(('f1', l, ei, q, i))
                            wv = slot[:, 0:2 * 8 * 256].rearrange("p (g k n) -> p g k n", g=2, k=8)
                            for fi in range(2):
                                for j, (t0, tn) in enumerate(TILES):
                                    b1 = palloc(1)
                                    b3 = palloc(1)
                                    for g, b in ((0, b1), (1, b3)):
                                        for k in range(8):
                                            pe(lambda e, b=b, g=g, k=k, fi=fi, t0=t0, tn=tn, wv=wv: e.matmul(
                                                ps[:, b, 0:tn], lhsT=wv[:, g, k, fi * 128:(fi + 1) * 128], rhs=ubuf[:, k, t0:t0 + tn],
                                                start=(k == 0), stop=(k == 7)), [skey, ('u', k, j, 0), ('u', k, j, 1)], pk(b))
                                    ts_ = tS[(fi * 3 + j) % 2]
                                    tsk = ('tS', (fi * 3 + j) % 2)
                                    act(lambda e, b1=b1, ts_=ts_, tn=tn: e.activation(out=ts_[:, 0:tn], in_=ps[:, b1, 0:tn], func=AF.Silu), pk(b1), [tsk])
                                    dve(lambda e, b3=b3, ts_=ts_, t0=t0, tn=tn, aq=aq, fl=i + fi: e.tensor_tensor(
                                        out=aq[:, fl, t0:t0 + tn], in0=ps[:, b3, 0:tn], in1=ts_[:, 0:tn], op=ALU.mult), pk(b3) + [tsk], [(aqk, i + fi, j)])
                            for (ml, n) in mod_jobs_at(('f1', l, ei, q, i)):
                                mod_job(ml, n)
                        slot, skey = ring_next(('f2', l, ei, q))
                        w2v = slot[:, 0:nf * 1024].rearrange("p (k n) -> p k n", k=nf)
                        for tb in range(NTB):
                            b = palloc(2)
                            for k in range(nf):
                                for hf in range(2):
                                    pe(lambda e, b=b, k=k, hf=hf, tb=tb, aq=aq, w2v=w2v: e.matmul(
                                        ps[:, b + hf, :], lhsT=aq[:, k, tb * 128:(tb + 1) * 128], rhs=w2v[:, k, hf * 512:(hf + 1) * 512],
                                        start=(k == 0), stop=(k == nf - 1)), [skey, (aqk, k, tb // 4)], pk(b + hf))
                            src = ps[:, b:b + 2, :].rearrange("p a b -> p (a b)")
                            if first:
                                if moe:
                                    dve(lambda e, src=src, tb=tb, ei=ei: e.tensor_scalar(out=f_acc[:, tb, :], in0=src, scalar1=comb[:, tb * 8 + ei:tb * 8 + ei + 1],
                                                                                         scalar2=None, op0=ALU.mult), pk(b, 2) + ['comb'], [('facc', tb)])
                                else:
                                    dve(lambda e, src=src, tb=tb: e.tensor_copy(out=f_acc[:, tb, :], in_=src), pk(b, 2), [('facc', tb)])
                            else:
                                sc_ = comb[:, tb * 8 + ei:tb * 8 + ei + 1] if moe else 1.0
                                dve(lambda e, src=src, tb=tb, sc_=sc_: e.scalar_tensor_tensor(out=f_acc[:, tb, :], in0=src, scalar=sc_, in1=f_acc[:, tb, :],
                                                                                             op0=ALU.mult, op1=ALU.add),
                                    pk(b, 2) + ['comb', ('facc', tb)], [('facc', tb)])
                            if lastq and tb in (3, 7, 9):
                                final_tile({3: 0, 7: 1, 9: 2}[tb])
                        first = False

        except _Stop:
            for tb in range(NTB):
                tk = P.dma('sp', 'sty', lambda e, tb=tb: e.dma_start(out=yout[tb * 128:(tb + 1) * 128, :], in_=xres[:, tb, :]), reads=[('x', tb)])
                out_toks.append(tk)

        tk = P.dma('sp', 'stst', lambda e: e.dma_start(out=stout, in_=stbuf[:]), reads=['stbuf'])
        out_toks.append(tk)
        final = {}
        for (n, v) in out_toks:
            final[n] = max(final.get(n, 0), v)
        for n, v in final.items():
            P.wait_tok('sp', (n, v))
        assert stop is not None or rst['used'] == len(pieces), (rst['used'], len(pieces))

        sems = {n: es.enter_context(nc.semaphore(n)) for n in P.sem_names()}
        with nc.Block() as block:
            @block.tensor
            def _(e):
                P.replay('pe', e, sems)

            @block.scalar
            def _(e):
                P.replay('act', e, sems)

            @block.vector
            def _(e):
                P.replay('dve', e, sems)

            @block.gpsimd
            def _(e):
                P.replay('pool', e, sems)

            @block.sync
            def _(e):
                P.replay('sp', e, sems)
    return nc


def _feat(v):
    v = np.asarray(v, np.float32)
    lead = v.shape[:-1]
    return np.moveaxis(v.reshape(lead + (8, 128)), -1, 0)


_NC_CACHE = {}


def kernel(x_prompt, x_sample, state_rglru, c, c_ctx, w_mod, b_mod, w_in, conv_a, w_a_out, conv_b, conv_b_bias,
           w_gate_a, b_gate_a, w_gate_x, b_gate_x, lru_lambda, w_b_out, w_o, ln1_g, ln1_b, ln2_g, ln2_b,
           ffn_w1, ffn_w3, ffn_w2, router_w, router_b, moe_w1, moe_w3, moe_w2):
    f32 = lambda a: np.ascontiguousarray(np.asarray(a, np.float32))
    x_prompt, x_sample, state_rglru, c, c_ctx = map(f32, (x_prompt, x_sample, state_rglru, c, c_ctx))
    ncores = 8
    smallp = np.zeros((128, NS), np.float32)
    smallp[:, SP_BMOD:SP_BMOD + 96] = np.moveaxis(f32(b_mod).reshape(L, 48, 128), -1, 0).reshape(128, 96)
    smallp[:, SP_CA:SP_CA + 48] = _feat(conv_a).reshape(128, 48)
    smallp[:, SP_CB:SP_CB + 64] = _feat(conv_b).reshape(128, 64)
    smallp[:, SP_CBB:SP_CBB + 16] = _feat(conv_b_bias).reshape(128, 16)
    smallp[:, SP_BGA:SP_BGA + 32] = _feat(b_gate_a).reshape(128, 32)
    smallp[:, SP_BGX:SP_BGX + 32] = _feat(b_gate_x).reshape(128, 32)
    smallp[:, SP_LAM:SP_LAM + 32] = _feat(lru_lambda).reshape(128, 32)
    smallp[:, SP_RB:SP_RB + 8] = np.broadcast_to(f32(router_b)[0][None, :], (128, 8))
    lnp = np.stack([f32(ln1_g), f32(ln1_b), f32(ln2_g), f32(ln2_b)], axis=1)
    gwbd = np.zeros((L, 4, 8, 128, 128), np.float32)
    wga, wgx = f32(w_gate_a), f32(w_gate_x)
    for d in range(2):
        for g, wsrc in enumerate((wga, wgx)):
            for h in range(16):
                cc, hh = h // 2, h % 2
                gwbd[:, d * 2 + g, cc, hh * 64:(hh + 1) * 64, hh * 64:(hh + 1) * 64] = wsrc[:, d, h]
    ident = np.eye(128, dtype=np.float32)
    shared = dict(smallp=smallp, lnp=lnp, gwbd=gwbd, ident=ident, w_mod=f32(w_mod), w_in=f32(w_in), w_a_out=f32(w_a_out),
                  w_b_out=f32(w_b_out), w_o=f32(w_o), ffn_w1=f32(ffn_w1), ffn_w3=f32(ffn_w3), ffn_w2=f32(ffn_w2),
                  router_w=f32(router_w), moe_w1=f32(moe_w1), moe_w3=f32(moe_w3), moe_w2=f32(moe_w2))
    in_maps = []
    seqmap = []
    for core in range(ncores):
        flags = np.zeros((128, NFL), np.float32)
        h0 = np.zeros((128, L, 2, 8, 5), np.float32)
        cf = np.zeros(20, np.float32)
        if core < 2:
            b = core
            xin = np.concatenate([x_sample[b], x_prompt[b]], axis=0)
            conds = np.stack([c[b], c_ctx], axis=0)
            flags[:, FL_KF + 1:FL_KF + 4] = 1.0
            flags[:, FL_KB + 0:FL_KB + 3] = 1.0
            cf[17:20] = 1.0
            st = _feat(state_rglru[b])
            h0[:, :, 0, :, 0] = st[:, :, 0, :]
            h0[:, :, 1, :, 3] = st[:, :, 1, :]
            seqmap.append([None, None, None, None, b])
        else:
            s0 = 2 + 5 * (core - 2)
            xin = x_prompt[s0:s0 + 5].reshape(NTOK, D)
            conds = np.stack([c_ctx, c_ctx], axis=0)
            for r in range(20):
                cf[r] = 1.0 if r % 4 != 0 else 0.0
            seqmap.append([s0 + i for i in range(5)])
        flags[:, FL_CF:FL_CF + 20] = cf[None, :]
        cond = np.ascontiguousarray(np.moveaxis(conds.reshape(2, 8, 128), -1, 0).transpose(0, 2, 1)).reshape(128, 16)
        m = dict(shared)
        m.update(xin=np.ascontiguousarray(xin), cond=cond, h0=h0.reshape(128, 160), flags=flags)
        in_maps.append(m)
    if 'nc' not in _NC_CACHE:
        _NC_CACHE['nc'] = build_nc()
    res = run_bass_kernel_spmd(_NC_CACHE['nc'], in_maps, core_ids=list(range(ncores)))
    y_prompt = np.zeros((32, 256, D), np.float32)
    y_sample = np.zeros((2, 1024, D), np.float32)
    new_state = np.zeros((32, L, 2, D), np.float32)
    for core in range(ncores):
        y = res.results[core]["yout"]
        st = res.results[core]["stout"].reshape(128, L, 2, 8, 5)
        if core < 2:
            y_sample[core] = y[0:1024]
        for s, seq in enumerate(seqmap[core]):
            if seq is None:
                continue
            y_prompt[seq] = y[s * 256:(s + 1) * 256]
            new_state[seq] = np.moveaxis(st[:, :, :, :, s], 0, -1).reshape(L, 2, D)
    return (y_prompt, y_sample, new_state)
```
